# Optimizing a Trainium2 kernel written in Bass

```python
import jax, jax.numpy as jnp
from jax import lax
import numpy as np

D_MODEL = 1024
BATCH = 8
SEQ = 4096
DEPTH = 2

D_MIX = D_MODEL
D_FF = 2816
RMS_EPS = 1e-6
D_SSD = D_MIX // 2
SSD_HEAD_DIM = 64
SSD_HEADS = D_SSD // SSD_HEAD_DIM
SSD_GROUPS = 2
SSD_STATE = 128
SSD_CONV = 4
SSD_CHUNK = 128
SSD_CONV_CH = D_SSD + 2 * SSD_GROUPS * SSD_STATE
D_ATT = D_MIX // 4
ATT_HEAD_DIM = 64
ATT_HEADS = D_ATT // ATT_HEAD_DIM
Q_RANK = 256
KV_RANK = 128
IDX_HEADS = 4
IDX_DIM = 64
TOP_K = 256
Q_BLOCK = 128
ROPE_THETA = 500000.0
ROPE_FRACTION_DEN = 4
D_LRU = D_MIX - D_SSD - D_ATT
LRU_BLOCKS = 4
LRU_BLOCK_W = D_LRU // LRU_BLOCKS
LRU_CONV = 4
LRU_C = 8.0
SSD_IN = D_SSD + SSD_CONV_CH + SSD_HEADS
ATT_IN = Q_RANK + KV_RANK + IDX_DIM + IDX_HEADS
LRU_IN = 2 * D_LRU
IN_COLS = SSD_IN + ATT_IN + LRU_IN

kernel_name = 'hymba_style_ssd_dsa_rglru_macaron'


def _in_proj_offsets():
    sizes = [D_SSD, SSD_CONV_CH, SSD_HEADS, Q_RANK, KV_RANK, IDX_DIM, IDX_HEADS, D_LRU, D_LRU]
    return np.cumsum(sizes)[:-1].tolist()


def rms_norm(x, g):
    xf = x.astype(jnp.float32)
    y = xf * lax.rsqrt(jnp.mean(xf * xf, axis=-1, keepdims=True) + RMS_EPS)
    return (y * g.astype(jnp.float32)).astype(x.dtype)


def swiglu(h, w13, w2):
    g, u = jnp.split(h @ w13, 2, axis=-1)
    return (jax.nn.silu(g) * u) @ w2


def causal_dwconv(x, w, b):
    width, ch = w.shape
    y = lax.conv_general_dilated(x, w[:, None, :].astype(x.dtype), window_strides=(1,),
                                 padding=[(width - 1, 0)], dimension_numbers=('NWC', 'WIO', 'NWC'),
                                 feature_group_count=ch)
    return y + b


def rope_tables(length, rot_dim, dtype):
    half = rot_dim // 2
    inv = ROPE_THETA ** (-jnp.arange(half, dtype=jnp.float32) * 2.0 / rot_dim)
    ang = jnp.arange(length, dtype=jnp.float32)[:, None] * inv[None, :]
    return jnp.cos(ang).astype(dtype), jnp.sin(ang).astype(dtype)


def partial_rope(x, cos, sin):
    half = cos.shape[-1]
    x1, x2, rest = x[..., :half], x[..., half:2 * half], x[..., 2 * half:]
    return jnp.concatenate([x1 * cos - x2 * sin, x1 * sin + x2 * cos, rest], axis=-1)


def ssd_mixer(z, xbc, dt_raw, conv_w, conv_b, dt_bias, a_log, d_skip, norm_g):
    bsz, length, _ = z.shape
    nc = length // SSD_CHUNK
    rpg = SSD_HEADS // SSD_GROUPS
    xbc = jax.nn.silu(causal_dwconv(xbc, conv_w, conv_b))
    xs, bm, cm = jnp.split(xbc, [D_SSD, D_SSD + SSD_GROUPS * SSD_STATE], axis=-1)
    xs = xs.reshape(bsz, nc, SSD_CHUNK, SSD_GROUPS, rpg, SSD_HEAD_DIM)
    bm = bm.reshape(bsz, nc, SSD_CHUNK, SSD_GROUPS, SSD_STATE)
    cm = cm.reshape(bsz, nc, SSD_CHUNK, SSD_GROUPS, SSD_STATE)
    dt = jax.nn.softplus((dt_raw + dt_bias).astype(jnp.float32))
    a = -jnp.exp(a_log.astype(jnp.float32))
    adt = (dt * a).reshape(bsz, nc, SSD_CHUNK, SSD_GROUPS, rpg)
    dt = dt.reshape(bsz, nc, SSD_CHUNK, SSD_GROUPS, rpg)
    acum = jnp.moveaxis(jnp.cumsum(adt, axis=2), 2, -1)
    causal = jnp.tril(jnp.ones((SSD_CHUNK, SSD_CHUNK), dtype=bool))
    seg = acum[..., :, None] - acum[..., None, :]
    decay = jnp.exp(jnp.where(causal, seg, -jnp.inf))
    cb = jnp.einsum('bcign,bcjgn->bcgij', cm, bm)
    y_diag = jnp.einsum('bcgij,bcgrij,bcjgr,bcjgrp->bcigrp', cb, decay, dt, xs)
    decay_last = jnp.exp(acum[..., -1:] - acum)
    states = jnp.einsum('bcjgn,bcgrj,bcjgr,bcjgrp->bcgrpn', bm, decay_last, dt, xs)
    chunk_decay = jnp.exp(acum[..., -1])

    def step(h, inp):
        dec, st = inp
        return dec[..., None, None] * h + st, h

    h0 = jnp.zeros((bsz, SSD_GROUPS, rpg, SSD_HEAD_DIM, SSD_STATE), states.dtype)
    _, prev = lax.scan(step, h0, (jnp.moveaxis(chunk_decay, 1, 0), jnp.moveaxis(states, 1, 0)))
    prev = jnp.moveaxis(prev, 0, 1)
    y_off = jnp.einsum('bcign,bcgrpn,bcgri->bcigrp', cm, prev, jnp.exp(acum))
    y = y_diag + y_off + d_skip.reshape(SSD_GROUPS, rpg)[:, :, None] * xs
    y = y.reshape(bsz, length, D_SSD).astype(z.dtype)
    yg = (y * jax.nn.silu(z)).reshape(bsz, length, SSD_GROUPS, D_SSD // SSD_GROUPS)
    return rms_norm(yg, norm_g.reshape(SSD_GROUPS, -1)).reshape(bsz, length, D_SSD)


def dsa_mixer(cq, ckv, kidx, widx, cq_g, ckv_g, w_uq, w_ukv, q_g, k_g, w_qidx, kidx_g):
    bsz, length, _ = cq.shape
    cq = rms_norm(cq, cq_g)
    ckv = rms_norm(ckv, ckv_g)
    q = (cq @ w_uq).reshape(bsz, length, ATT_HEADS, ATT_HEAD_DIM)
    kv = (ckv @ w_ukv).reshape(bsz, length, ATT_HEADS, 2 * ATT_HEAD_DIM)
    k, v = jnp.split(kv, 2, axis=-1)
    cos, sin = rope_tables(length, ATT_HEAD_DIM // ROPE_FRACTION_DEN, q.dtype)
    q = partial_rope(rms_norm(q, q_g), cos[:, None], sin[:, None])
    k = partial_rope(rms_norm(k, k_g), cos[:, None], sin[:, None])
    ci, si = rope_tables(length, IDX_DIM // ROPE_FRACTION_DEN, q.dtype)
    qi = partial_rope((cq @ w_qidx).reshape(bsz, length, IDX_HEADS, IDX_DIM), ci[:, None], si[:, None])
    ki = partial_rope(rms_norm(kidx, kidx_g), ci, si)
    widx = widx * IDX_HEADS ** -0.5
    n_sel = min(TOP_K, length // 4)
    nb = length // Q_BLOCK
    key_pos = jnp.arange(length)
    gather = jax.vmap(lambda tab, ids: tab[ids])

    def to_blocks(t):
        return jnp.moveaxis(t.reshape(bsz, nb, Q_BLOCK, *t.shape[2:]), 1, 0)

    def block(args):
        qb, qib, wb, start = args
        qpos = start + jnp.arange(Q_BLOCK)
        rel = jax.nn.relu(jnp.einsum('bqhd,bsd->bqhs', qib, ki).astype(jnp.float32) * IDX_DIM ** -0.5)
        score = jnp.einsum('bqhs,bqh->bqs', rel, wb.astype(jnp.float32))
        score = jnp.where(key_pos[None, :] <= qpos[:, None], score, -jnp.inf)
        _, idx = lax.top_k(score, n_sel)
        ks = gather(k, idx)
        vs = gather(v, idx)
        logits = jnp.einsum('bqhd,bqkhd->bqhk', qb, ks).astype(jnp.float32) * ATT_HEAD_DIM ** -0.5
        valid = (idx <= qpos[None, :, None])[:, :, None, :]
        p = jax.nn.softmax(jnp.where(valid, logits, -jnp.inf), axis=-1)
        return jnp.einsum('bqhk,bqkhd->bqhd', p.astype(vs.dtype), vs)

    starts = jnp.arange(nb) * Q_BLOCK
    out = lax.map(block, (to_blocks(q), to_blocks(qi), to_blocks(widx), starts))
    return jnp.moveaxis(out, 0, 1).reshape(bsz, length, D_ATT)


def rglru_mixer(xb, gb, conv_w, conv_b, wa, ba, wi, bi, lam):
    bsz, length, _ = xb.shape
    xr = causal_dwconv(xb, conv_w, conv_b)
    xblk = xr.reshape(bsz, length, LRU_BLOCKS, LRU_BLOCK_W)
    r = jax.nn.sigmoid(jnp.einsum('blnc,ncd->blnd', xblk, wa).reshape(bsz, length, D_LRU) + ba)
    i = jax.nn.sigmoid(jnp.einsum('blnc,ncd->blnd', xblk, wi).reshape(bsz, length, D_LRU) + bi)
    log_a = -LRU_C * r.astype(jnp.float32) * jax.nn.softplus(-lam.astype(jnp.float32))
    a = jnp.exp(log_a)
    b = jnp.sqrt(-jnp.expm1(2.0 * log_a)) * (i * xr).astype(jnp.float32)

    def combine(left, right):
        a_l, b_l = left
        a_r, b_r = right
        return a_l * a_r, a_r * b_l + b_r

    _, hs = lax.associative_scan(combine, (a, b), axis=1)
    return hs.astype(xb.dtype) * jax.nn.gelu(gb)


def setup_inputs(seed: int = 0) -> dict:
    key = jax.random.key(seed)
    ks = jax.random.split(key, 32)
    f32 = jnp.float32

    def w(k, shape, fan_in):
        return jax.random.normal(k, shape, f32) * fan_in ** -0.5

    def gain(k, shape):
        return 1.0 + 0.02 * jax.random.normal(k, shape, f32)

    def small(k, shape):
        return 0.02 * jax.random.normal(k, shape, f32)

    L = DEPTH
    dt0 = jnp.exp(jax.random.uniform(ks[8], (L, SSD_HEADS), f32, np.log(1e-3), np.log(1e-1)))
    a_pow = jax.random.uniform(ks[26], (L, D_LRU), f32, 0.9, 0.999)
    sig = a_pow ** (1.0 / LRU_C)
    return {
        'x': jax.random.normal(ks[0], (BATCH, SEQ, D_MODEL), f32),
        'norm_ffn1': gain(ks[1], (L, D_MODEL)),
        'ffn1_w13': w(ks[2], (L, D_MODEL, 2 * D_FF), D_MODEL),
        'ffn1_w2': w(ks[3], (L, D_FF, D_MODEL), D_FF),
        'norm_mix': gain(ks[4], (L, D_MODEL)),
        'w_in': w(ks[5], (L, D_MODEL, IN_COLS), D_MODEL),
        'ssd_conv_w': w(ks[6], (L, SSD_CONV, SSD_CONV_CH), SSD_CONV),
        'ssd_conv_b': small(ks[7], (L, SSD_CONV_CH)),
        'ssd_dt_bias': dt0 + jnp.log(-jnp.expm1(-dt0)),
        'ssd_a_log': jnp.log(jax.random.uniform(ks[9], (L, SSD_HEADS), f32, 1.0, 16.0)),
        'ssd_d': gain(ks[10], (L, SSD_HEADS)),
        'ssd_norm': gain(ks[11], (L, D_SSD)),
        'cq_norm': gain(ks[12], (L, Q_RANK)),
        'ckv_norm': gain(ks[13], (L, KV_RANK)),
        'w_uq': w(ks[14], (L, Q_RANK, ATT_HEADS * ATT_HEAD_DIM), Q_RANK),
        'w_ukv': w(ks[15], (L, KV_RANK, ATT_HEADS * 2 * ATT_HEAD_DIM), KV_RANK),
        'q_norm': gain(ks[16], (L, ATT_HEAD_DIM)),
        'k_norm': gain(ks[17], (L, ATT_HEAD_DIM)),
        'w_qidx': w(ks[18], (L, Q_RANK, IDX_HEADS * IDX_DIM), Q_RANK),
        'kidx_norm': gain(ks[19], (L, IDX_DIM)),
        'lru_conv_w': w(ks[20], (L, LRU_CONV, D_LRU), LRU_CONV),
        'lru_conv_b': small(ks[21], (L, D_LRU)),
        'lru_wa': w(ks[22], (L, LRU_BLOCKS, LRU_BLOCK_W, LRU_BLOCK_W), LRU_BLOCK_W),
        'lru_ba': small(ks[23], (L, D_LRU)),
        'lru_wi': w(ks[24], (L, LRU_BLOCKS, LRU_BLOCK_W, LRU_BLOCK_W), LRU_BLOCK_W),
        'lru_bi': small(ks[25], (L, D_LRU)),
        'lru_lambda': jnp.log(sig / (1.0 - sig)),
        'w_out': w(ks[27], (L, D_MIX, D_MODEL), D_MIX),
        'norm_ffn2': gain(ks[28], (L, D_MODEL)),
        'ffn2_w13': w(ks[29], (L, D_MODEL, 2 * D_FF), D_MODEL),
        'ffn2_w2': w(ks[30], (L, D_FF, D_MODEL), D_FF),
    }


def reference(x, norm_ffn1, ffn1_w13, ffn1_w2, norm_mix, w_in, ssd_conv_w, ssd_conv_b, ssd_dt_bias,
              ssd_a_log, ssd_d, ssd_norm, cq_norm, ckv_norm, w_uq, w_ukv, q_norm, k_norm, w_qidx,
              kidx_norm, lru_conv_w, lru_conv_b, lru_wa, lru_ba, lru_wi, lru_bi, lru_lambda, w_out,
              norm_ffn2, ffn2_w13, ffn2_w2):
    offsets = _in_proj_offsets()
    for l in range(DEPTH):
        x = x + 0.5 * swiglu(rms_norm(x, norm_ffn1[l]), ffn1_w13[l], ffn1_w2[l])
        h = rms_norm(x, norm_mix[l])
        proj = h @ w_in[l]
        z, xbc, dt_raw, cq, ckv, kidx, widx, xl, gl = jnp.split(proj, offsets, axis=-1)
        y_ssd = ssd_mixer(z, xbc, dt_raw, ssd_conv_w[l], ssd_conv_b[l], ssd_dt_bias[l], ssd_a_log[l],
                          ssd_d[l], ssd_norm[l])
        y_att = dsa_mixer(cq, ckv, kidx, widx, cq_norm[l], ckv_norm[l], w_uq[l], w_ukv[l], q_norm[l],
                          k_norm[l], w_qidx[l], kidx_norm[l])
        y_lru = rglru_mixer(xl, gl, lru_conv_w[l], lru_conv_b[l], lru_wa[l], lru_ba[l], lru_wi[l],
                            lru_bi[l], lru_lambda[l])
        mixed = jnp.concatenate([y_ssd, y_att, y_lru], axis=-1)
        x = x + mixed @ w_out[l]
        x = x + 0.5 * swiglu(rms_norm(x, norm_ffn2[l]), ffn2_w13[l], ffn2_w2[l])
    return x
```

```python
import numpy as np
import concourse.bass as bass
import concourse.mybir as mybir
from concourse.bass_utils import run_bass_kernel_spmd

F32 = mybir.dt.float32
BF16 = mybir.dt.bfloat16
AF = mybir.ActivationFunctionType
ALU = mybir.AluOpType
AX = mybir.AxisListType

D_MODEL = 1024
SEQ = 4096
DEPTH = 2
D_FF = 2816
NCORES = 8
EPS = 1e-6
IN_COLS = 2508


class Buf:
    __slots__ = ("name", "w", "r")

    def __init__(self, name):
        self.name = name
        self.w = None
        self.r = {}


class Prog:
    ENG = ["pe", "act", "dve", "pool", "sp"]
    NDS = 40

    def __init__(self, nc):
        self.nc = nc
        self.eng = dict(pe=nc.tensor, act=nc.scalar, dve=nc.vector, pool=nc.gpsimd, sp=nc.sync)
        self.sem = {e: nc.alloc_semaphore("sem_" + e) for e in self.ENG}
        self.cnt = {e: 0 for e in self.ENG}
        self.dsem = [nc.alloc_semaphore(f"dmas{i}") for i in range(self.NDS)]
        self.dval = [0] * self.NDS
        self.dnext = 0
        self.seen = {e: {} for e in self.ENG}
        self.nwait = 0
        self.nins = 0

    def _wait(self, e, tok):
        kind, idx, val = tok
        if kind == "e" and idx == e and e == "pe":
            return
        key = (kind, idx)
        if self.seen[e].get(key, 0) >= val:
            return
        sem = self.sem[idx] if kind == "e" else self.dsem[idx]
        self.eng[e].wait_ge(sem, val)
        self.seen[e][key] = val
        self.nwait += 1

    def _deps(self, e, reads, writes):
        for b in reads:
            if b.w is not None:
                self._wait(e, b.w)
        for b in writes:
            if b.w is not None:
                self._wait(e, b.w)
            for k, v in b.r.items():
                self._wait(e, (k[0], k[1], v))

    def _mark(self, tok, reads, writes):
        key = (tok[0], tok[1])
        for b in reads:
            if b.r.get(key, 0) < tok[2]:
                b.r[key] = tok[2]
        for b in writes:
            b.w = tok
            b.r = {}

    def op(self, e, fn, reads=(), writes=(), inc=True):
        self._deps(e, reads, writes)
        ins = fn(self.eng[e])
        self.nins += 1
        if inc:
            ins.then_inc(self.sem[e], 1)
            self.cnt[e] += 1
            tok = ("e", e, self.cnt[e])
        else:
            assert e == "pe"
            tok = ("e", e, self.cnt[e] + 1)
        self._mark(tok, reads, writes)
        return ins

    def dma(self, q, out, in_, reads=(), writes=(), **kw):
        self._deps(q, reads, writes)
        s = self.dnext
        self.dnext = (self.dnext + 1) % self.NDS
        if self.dval[s] > 0:
            self._wait(q, ("d", s, self.dval[s]))
        ins = self.eng[q].dma_start(out=out, in_=in_, **kw)
        ins.then_inc(self.dsem[s], 16)
        self.dval[s] += 16
        self.nins += 1
        self._mark(("d", s, self.dval[s]), reads, writes)
        return ins

    def barrier(self):
        for e in self.ENG:
            for e2 in self.ENG:
                if e2 != e and self.cnt[e2] > 0:
                    self._wait(e, ("e", e2, self.cnt[e2]))
            for s in range(self.NDS):
                if self.dval[s] > 0:
                    self._wait(e, ("d", s, self.dval[s]))


class Ctx:
    def __init__(self, nc):
        self.nc = nc
        self.stack = []
        self.n = 0

    def sb(self, shape, dt, name=None):
        self.n += 1
        g = self.nc.sbuf_tensor(f"{name or 't'}_{self.n}", list(shape), dt)
        t = g.__enter__()
        self.stack.append(g)
        return t

    def ps(self, shape, dt=F32, name=None):
        self.n += 1
        g = self.nc.psum_tensor(f"{name or 'p'}_{self.n}", list(shape), dt)
        t = g.__enter__()
        self.stack.append(g)
        return t

    def mark(self):
        return len(self.stack)

    def release(self, m):
        while len(self.stack) > m:
            g = self.stack.pop()
            g.__exit__(None, None, None)


def ffn_phase(P, C, K, l, which, src_tm=None, dst_tm=None, outproj=False):
    nc = P.nc
    TT = 256
    NT = SEQ // TT
    m = C.mark()
    w13_d = K[f"ffn{which}_w13"][l]
    w2_d = K[f"ffn{which}_w2"][l]
    g_d = K[f"norm_ffn{which}"][l]
    xT = K["xT"]

    w13 = C.sb([128, 8, 2 * D_FF], BF16, "w13")
    w2 = C.sb([128, 22, D_MODEL], BF16, "w2")
    g_sb = C.sb([128, 8], F32, "g")
    b_w13 = [Buf(f"w13_{c}") for c in range(8)]
    b_w2 = [Buf(f"w2_{j}") for j in range(22)]
    b_g = Buf("g")
    for c in range(8):
        P.dma("pool", w13[:, c, :], w13_d[:, c, :], writes=[b_w13[c]])
    for j0 in range(0, 22, 2):
        P.dma("pool", w2[:, j0:j0 + 2, :], w2_d[:, j0:j0 + 2, :], writes=[b_w2[j0], b_w2[j0 + 1]])
    P.dma("sp", g_sb[:], g_d[:, :], writes=[b_g])

    if outproj:
        wo = C.sb([128, 8, D_MODEL], BF16, "wo")
        b_wo = Buf("wo")
        P.dma("sp", wo[:], K["w_out_s"][:, :, :], writes=[b_wo])
        mx = [C.sb([128, 8, TT], BF16, "mx") for _ in range(2)]
        b_mx = [Buf("mx0"), Buf("mx1")]
        mixed_v = K["mixedT"].rearrange("(c p) t -> p c t", p=128)
    x_sb = [C.sb([128, 8, TT], F32, "x") for _ in range(2)]
    b_x = [Buf("x0"), Buf("x1")]
    xn = C.sb([128, 8, TT], BF16, "xn")
    b_xn = Buf("xn")
    hT = C.sb([128, 22, TT], BF16, "hT")
    b_h = [Buf(f"h{j}") for j in range(22)]
    sg = [C.sb([128, TT], F32, "sg") for _ in range(2)]
    b_sg = [Buf("sg0"), Buf("sg1")]
    rstd = C.sb([128, TT], F32, "rstd")
    b_rstd = Buf("rstd")
    ps_ss = C.ps([128, 512], F32, "ss")
    b_ss = Buf("ss")
    ps_g = [C.ps([128, 512], F32, "pg") for _ in range(2)]
    ps_u = [C.ps([128, 512], F32, "pu") for _ in range(2)]
    b_pg = [Buf("pg0"), Buf("pg1")]
    b_pu = [Buf("pu0"), Buf("pu1")]
    ps_o = [C.ps([128, 512], F32, "po") for _ in range(2)]
    b_po = [Buf("po0"), Buf("po1")]
    if src_tm is not None:
        xtm = C.sb([128, 2, D_MODEL], F32, "xtm")
        b_xtm = Buf("xtm")
    if dst_tm is not None:
        ytm = C.sb([128, 2, D_MODEL], F32, "ytm")
        b_ytm = Buf("ytm")
    ones_bf = K["ones_bf"]
    ident_f = K["ident_f"]
    eps_sb = K["eps_sb"]
    b_const = K["b_const"]
    oc = 0
    for it in range(NT):
        t0 = it * TT
        xb = x_sb[it % 2]
        bx = b_x[it % 2]
        if src_tm is None:
            P.dma("sp", xb[:], xT[:, :, t0:t0 + TT].rearrange("c p t -> p c t"), writes=[bx])
        else:
            P.dma("sp", xtm[:], src_tm[t0:t0 + TT, :].rearrange("(s p) d -> p s d", p=128), writes=[b_xtm])
            for c2 in range(4):
                pst = ps_o[oc % 2]
                bpo = b_po[oc % 2]
                oc += 1
                for cc in range(2):
                    c = c2 * 2 + cc
                    for s in range(2):
                        P.op("pe", lambda e, c=c, s=s, cc=cc, pst=pst: e.transpose(
                            pst[:, cc * 256 + s * 128: cc * 256 + (s + 1) * 128], xtm[:, s, c * 128:(c + 1) * 128], ident_f[:]),
                            reads=[b_xtm, b_const], writes=[bpo], inc=(cc == 1 and s == 1))
                P.op("act", lambda e, c2=c2, pst=pst: e.copy(out=xb[:, c2 * 2:c2 * 2 + 2, :], in_=pst[:, 0:512].rearrange("p (c t) -> p c t", c=2)),
                     reads=[bpo], writes=[bx])
        if outproj:
            mxb, bmx = mx[it % 2], b_mx[it % 2]
            P.dma("sp", mxb[:], mixed_v[:, :, t0:t0 + TT], writes=[bmx])
            for dc in range(8):
                po = ps_o[oc % 2]
                bpo = b_po[oc % 2]
                oc += 1
                for c in range(8):
                    P.op("pe", lambda e, c=c, dc=dc, po=po, mxb=mxb: e.matmul(po[:, 0:TT], lhsT=wo[:, c, dc * 128:(dc + 1) * 128], rhs=mxb[:, c, :],
                                                                           start=(c == 0), stop=(c == 7)),
                         reads=[bmx, b_wo], writes=[bpo], inc=(c == 7))
                P.op("dve", lambda e, dc=dc, po=po: e.tensor_tensor(out=xb[:, dc, :], in0=xb[:, dc, :], in1=po[:, 0:TT], op=ALU.add),
                     reads=[bpo, bx], writes=[bx])
        rms_xn(P, K, xb, bx, g_sb, b_g, xn, b_xn, rstd, b_rstd, ps_ss, b_ss, TT)
        for j in range(22):
            pg, pu = ps_g[j % 2], ps_u[j % 2]
            bg, bu = b_pg[j % 2], b_pu[j % 2]
            for c in range(8):
                P.op("pe", lambda e, c=c, j=j, pg=pg: e.matmul(pg[:, 0:TT], lhsT=w13[:, c, j * 128:(j + 1) * 128], rhs=xn[:, c, :],
                                                            start=(c == 0), stop=(c == 7)),
                     reads=[b_xn, b_w13[c]], writes=[bg], inc=(c == 7))
            for c in range(8):
                P.op("pe", lambda e, c=c, j=j, pu=pu: e.matmul(pu[:, 0:TT], lhsT=w13[:, c, D_FF + j * 128:D_FF + (j + 1) * 128], rhs=xn[:, c, :],
                                                            start=(c == 0), stop=(c == 7)),
                     reads=[b_xn, b_w13[c]], writes=[bu], inc=(c == 7))
            sgb, bsg = sg[j % 2], b_sg[j % 2]
            P.op("act", lambda e, pg=pg, sgb=sgb: e.activation(out=sgb[:], in_=pg[:, 0:TT], func=AF.Silu), reads=[bg], writes=[bsg])
            P.op("dve", lambda e, j=j, pu=pu, sgb=sgb: e.tensor_tensor(out=hT[:, j, :], in0=sgb[:], in1=pu[:, 0:TT], op=ALU.mult),
                 reads=[bsg, bu], writes=[b_h[j]])
        for dc in range(8):
            po = ps_o[oc % 2]
            bpo = b_po[oc % 2]
            oc += 1
            for j in range(22):
                P.op("pe", lambda e, j=j, dc=dc, po=po: e.matmul(po[:, 0:TT], lhsT=w2[:, j, dc * 128:(dc + 1) * 128], rhs=hT[:, j, :],
                                                              start=(j == 0), stop=(j == 21)),
                     reads=[b_h[j], b_w2[j]], writes=[bpo], inc=(j == 21))
            P.op("dve", lambda e, dc=dc, po=po: e.scalar_tensor_tensor(out=xb[:, dc, :], in0=po[:, 0:TT], scalar=0.5, in1=xb[:, dc, :],
                                                                    op0=ALU.mult, op1=ALU.add),
                 reads=[bpo, bx], writes=[bx])
        if dst_tm is None:
            P.dma("sp", xT[:, :, t0:t0 + TT].rearrange("c p t -> p c t"), xb[:], reads=[bx])
        else:
            for c2 in range(4):
                pst = ps_o[oc % 2]
                bpo = b_po[oc % 2]
                oc += 1
                for s in range(2):
                    for cc in range(2):
                        c = c2 * 2 + cc
                        P.op("pe", lambda e, c=c, s=s, cc=cc, pst=pst: e.transpose(
                            pst[:, s * 256 + cc * 128: s * 256 + (cc + 1) * 128], xb[:, c, s * 128:(s + 1) * 128], ident_f[:]),
                            reads=[bx, b_const], writes=[bpo], inc=(cc == 1 and s == 1))
                P.op("act", lambda e, c2=c2, pst=pst: e.copy(out=ytm[:, :, c2 * 256:(c2 + 1) * 256], in_=pst[:, 0:512].rearrange("p (s d) -> p s d", s=2)),
                     reads=[bpo], writes=[b_ytm])
            P.dma("sp", dst_tm[t0:t0 + TT, :].rearrange("(s p) d -> p s d", p=128), ytm[:], reads=[b_ytm])
    P.barrier()
    C.release(m)


def load_cast_scaled(P, C, dst_bf, b_dst, src_d, shape, scale_sb=None, b_scale=None, nchunk=None):
    if scale_sb is None:
        P.dma("pool", dst_bf, src_d, writes=[b_dst])
        return
    m = C.mark()
    st = C.sb(shape, F32, "wst")
    bs = Buf("wst")
    P.dma("sp", st[:], src_d, writes=[bs])
    if nchunk is None:
        P.op("dve", lambda e: e.tensor_scalar(out=dst_bf, in0=st[:], scalar1=scale_sb[:, 0:1], scalar2=None, op0=ALU.mult),
             reads=[bs, b_scale], writes=[b_dst])
    else:
        for c in range(nchunk):
            P.op("dve", lambda e, c=c: e.tensor_scalar(out=dst_bf[:, c, :], in0=st[:, c, :], scalar1=scale_sb[:, c:c + 1], scalar2=None, op0=ALU.mult),
                 reads=[bs, b_scale], writes=[b_dst])
    P.barrier()
    C.release(m)


def rms_xn(P, K, xb, bx, g_sb, b_g, xn, b_xn, rstd, b_rstd, ps_ss, b_ss, TT):
    ones_bf, eps_sb, b_const = K["ones_bf"], K["eps_sb"], K["b_const"]
    P.op("act", lambda e: e.activation(out=xn[:], in_=xb[:], func=AF.Square), reads=[bx], writes=[b_xn])
    for c in range(8):
        P.op("pe", lambda e, c=c: e.matmul(ps_ss[:, 0:TT], lhsT=ones_bf[:], rhs=xn[:, c, :], start=(c == 0), stop=(c == 7)),
             reads=[b_xn, b_const], writes=[b_ss], inc=(c == 7))
    P.op("act", lambda e: e.activation(out=rstd[:], in_=ps_ss[:, 0:TT], func=AF.Sqrt, bias=eps_sb[:, 0:1], scale=1.0 / D_MODEL),
         reads=[b_ss, b_const], writes=[b_rstd])
    P.op("dve", lambda e: e.reciprocal(out=rstd[:], in_=rstd[:]), reads=[b_rstd], writes=[b_rstd])
    for c in range(8):
        P.op("dve", lambda e, c=c: e.scalar_tensor_tensor(out=xn[:, c, :], in0=xb[:, c, :], scalar=g_sb[:, c:c + 1], in1=rstd[:],
                                                          op0=ALU.mult, op1=ALU.mult),
             reads=[bx, b_g, b_rstd], writes=[b_xn])


def inproj_phase(P, C, K, l):
    TT = 512
    NT = SEQ // TT
    m = C.mark()
    w_in = C.sb([128, 8, IN_COLS], BF16, "w_in")
    b_w = Buf("w_in")
    for c in range(8):
        P.dma("pool", w_in[:, c, :], K["w_in"][l][:, c, :], writes=[b_w])
    g_sb = C.sb([128, 8], F32, "g")
    b_g = Buf("g")
    P.dma("sp", g_sb[:], K["norm_mix"][l][:, :], writes=[b_g])
    x_sb = [C.sb([128, 8, TT], F32, "x") for _ in range(2)]
    b_x = [Buf("x0"), Buf("x1")]
    xn = C.sb([128, 8, TT], BF16, "xn")
    b_xn = Buf("xn")
    rstd = C.sb([128, TT], F32, "rstd")
    b_rstd = Buf("rstd")
    ps_ss = C.ps([128, 512], F32, "ss")
    b_ss = Buf("ss")
    ps_fm = [C.ps([128, 512], F32, "pfm") for _ in range(3)]
    b_pfm = [Buf(f"pfm{i}") for i in range(3)]
    ps_tm = [C.ps([128, 512], F32, "ptm") for _ in range(3)]
    b_ptm = [Buf(f"ptm{i}") for i in range(3)]
    NST = 4
    st_f = [C.sb([128, 512], F32, "stf") for _ in range(NST)]
    b_stf = [Buf(f"stf{i}") for i in range(NST)]
    st_b = [C.sb([128, 512], BF16, "stb") for _ in range(2)]
    b_stb = [Buf(f"stb{i}") for i in range(2)]
    xT = K["xT"]
    xbcT = K["xbcT"]
    cqkvT = K["cqkvT"]
    xlglT = K["xlglT"]
    fm = []
    for i in range(8):
        fm.append((512 + i * 128, xbcT, i * 128, False))
    for i in range(2):
        fm.append((1544 + i * 128, cqkvT, i * 128, True))
    fm.append((1800, cqkvT, 256, True))
    for i in range(4):
        fm.append((1996 + i * 128, xlglT, i * 128, False))
    fi = 0
    ti = 0
    si = 0
    sbi = 0
    for it in range(NT):
        t0 = it * TT
        xb, bx = x_sb[it % 2], b_x[it % 2]
        P.dma("sp", xb[:], xT[:, :, t0:t0 + TT].rearrange("c p t -> p c t"), writes=[bx])
        rms_xn(P, K, xb, bx, g_sb, b_g, xn, b_xn, rstd, b_rstd, ps_ss, b_ss, TT)
        for (col0, dst, row0, isb) in fm:
            ps, bp = ps_fm[fi % 3], b_pfm[fi % 3]
            fi += 1
            for c in range(8):
                P.op("pe", lambda e, c=c, ps=ps, col0=col0: e.matmul(ps[:, 0:TT], lhsT=w_in[:, c, col0:col0 + 128], rhs=xn[:, c, :],
                                                                  start=(c == 0), stop=(c == 7)),
                     reads=[b_xn, b_w], writes=[bp], inc=(c == 7))
            if isb:
                st, bs = st_b[sbi % 2], b_stb[sbi % 2]
                sbi += 1
                P.op("dve", lambda e, st=st, ps=ps: e.tensor_copy(out=st[:, 0:TT], in_=ps[:, 0:TT]), reads=[bp], writes=[bs])
            else:
                st, bs = st_f[si % NST], b_stf[si % NST]
                si += 1
                P.op("act", lambda e, st=st, ps=ps: e.copy(out=st[:, 0:TT], in_=ps[:, 0:TT]), reads=[bp], writes=[bs])
            P.dma("sp", dst[row0:row0 + 128, t0:t0 + TT], st[:, 0:TT], reads=[bs])
        for s in range(4):
            for (col0, ncol, dst) in ((0, 512, K["z_tm"]), (1536, 460, K["att_tm"])):
                ps, bp = ps_tm[ti % 3], b_ptm[ti % 3]
                ti += 1
                for c in range(8):
                    P.op("pe", lambda e, c=c, ps=ps, col0=col0, ncol=ncol, s=s: e.matmul(
                        ps[:, 0:ncol], lhsT=xn[:, c, s * 128:(s + 1) * 128], rhs=w_in[:, c, col0:col0 + ncol], start=(c == 0), stop=(c == 7)),
                        reads=[b_xn, b_w], writes=[bp], inc=(c == 7))
                st, bs = st_f[si % NST], b_stf[si % NST]
                si += 1
                if (ti % 2) == 0:
                    P.op("act", lambda e, st=st, ps=ps, ncol=ncol: e.copy(out=st[:, 0:ncol], in_=ps[:, 0:ncol]), reads=[bp], writes=[bs])
                else:
                    P.op("dve", lambda e, st=st, ps=ps, ncol=ncol: e.tensor_copy(out=st[:, 0:ncol], in_=ps[:, 0:ncol]), reads=[bp], writes=[bs])
                P.dma("sp", dst[t0 + s * 128:t0 + (s + 1) * 128, :], st[:, 0:ncol], reads=[bs])
    P.barrier()
    C.release(m)


def softplus_small(P, C, x, bx, shape, tmp_names="sp"):
    t = C.sb(shape, F32, "spt")
    bt = Buf("spt")
    P.op("act", lambda e: e.activation(out=t[:], in_=x, func=AF.Abs), reads=[bx], writes=[bt])
    P.op("act", lambda e: e.activation(out=t[:], in_=t[:], func=AF.Exp, scale=-1.0), reads=[bt], writes=[bt])
    P.op("act", lambda e: e.activation(out=t[:], in_=t[:], func=AF.Ln, bias=1.0), reads=[bt], writes=[bt])
    P.op("dve", lambda e: e.scalar_tensor_tensor(out=x, in0=x, scalar=0.0, in1=t[:], op0=ALU.max, op1=ALU.add), reads=[bx, bt], writes=[bx])


def ssd_phase(P, C, K, l):
    TT = 512
    NT = SEQ // TT
    m = C.mark()
    bc = K["b_const"]
    U, SL, ones_f, ident_b = K["U_f"], K["SL_f"], K["ones_f"], K["ident_b"]
    eps_sb = K["eps_sb"]
    m2 = C.mark()
    wo_f = C.sb([128, 8, D_MODEL], F32, "wo_f")
    wo_b = C.sb([128, 8, D_MODEL], BF16, "wo_b")
    gss = C.sb([128, 4], F32, "gss")
    b_wo = Buf("wo")
    P.dma("sp", wo_f[:], K["w_out"][l][:, :, :], writes=[b_wo])
    P.dma("sp", gss[:], K["ssd_norm"][l][:, :], writes=[b_wo])
    for c in range(8):
        if c < 4:
            P.op("dve", lambda e, c=c: e.tensor_scalar(out=wo_b[:, c, :], in0=wo_f[:, c, :], scalar1=gss[:, c:c + 1], scalar2=None, op0=ALU.mult),
                 reads=[b_wo], writes=[b_wo])
        else:
            P.op("act", lambda e, c=c: e.copy(out=wo_b[:, c, :], in_=wo_f[:, c, :]), reads=[b_wo], writes=[b_wo])
    P.dma("sp", K["w_out_s"][:, :, :], wo_b[:], reads=[b_wo])
    P.barrier()
    C.release(m2)

    cw = C.sb([128, 8, 4], F32, "cw")
    cb = C.sb([128, 8], F32, "cb")
    dtb = C.sb([128, 8], F32, "dtb")
    aneg = C.sb([128, 8], F32, "aneg")
    Dbc = C.sb([128, 512], F32, "Dbc")
    b_par = Buf("par")
    P.dma("sp", cw[:], K["ssd_conv_w"][l][:, :, :], writes=[b_par])
    P.dma("sp", cb[:], K["ssd_conv_b"][l][:, :], writes=[b_par])
    P.dma("sp", dtb[:], K["ssd_dt_bias"][l][:, :], writes=[b_par])
    P.dma("sp", aneg[:], K["ssd_a_log"][l][:, :], writes=[b_par])
    P.dma("sp", Dbc[:], K["ssd_d"][l][:, :], writes=[b_par])
    P.op("act", lambda e: e.activation(out=aneg[:], in_=aneg[:], func=AF.Exp), reads=[b_par], writes=[b_par])
    P.op("dve", lambda e: e.tensor_scalar(out=aneg[:], in0=aneg[:], scalar1=-1.0, scalar2=None, op0=ALU.mult), reads=[b_par], writes=[b_par])
    dt = C.sb([128, 32, 8], F32, "dt")
    adt = C.sb([128, 32, 8], F32, "adt")
    acum = C.sb([128, 32, 8], F32, "acum")
    tot = C.sb([128, 32, 8], F32, "tot")
    eacum = C.sb([128, 32, 8], F32, "eacum")
    dlast = C.sb([128, 32, 8], F32, "dlast")
    cdec = C.sb([128, 32, 8], F32, "cdec")
    b_dt = Buf("dt")
    b_su = Buf("su")
    for i in range(4):
        P.dma("sp", dt[:, i * 8:(i + 1) * 8, :], K["att_tm"][i * 1024:(i + 1) * 1024, 0:8].rearrange("(n p) h -> p n h", p=128), writes=[b_dt])
    P.op("dve", lambda e: e.tensor_tensor(out=dt[:], in0=dt[:], in1=dtb[:, :].unsqueeze(1).to_broadcast([128, 32, 8]), op=ALU.add),
         reads=[b_dt, b_par], writes=[b_dt])
    softplus_small(P, C, dt[:], b_dt, [128, 32, 8])
    P.op("dve", lambda e: e.tensor_tensor(out=adt[:], in0=dt[:], in1=aneg[:, :].unsqueeze(1).to_broadcast([128, 32, 8]), op=ALU.mult),
         reads=[b_dt, b_par], writes=[b_su])
    ps_seg = [C.ps([128, 512], F32, "pseg") for _ in range(2)]
    b_pseg = [Buf("pseg0"), Buf("pseg1")]
    adt2 = adt[:].rearrange("p n h -> p (n h)")
    P.op("pe", lambda e: e.matmul(ps_seg[0][:, 0:256], lhsT=U[:], rhs=adt2, start=True, stop=True), reads=[b_su, bc], writes=[b_pseg[0]])
    P.op("pe", lambda e: e.matmul(ps_seg[1][:, 0:256], lhsT=ones_f[:], rhs=adt2, start=True, stop=True), reads=[b_su, bc], writes=[b_pseg[1]])
    b_ac = Buf("acum")
    P.op("dve", lambda e: e.tensor_copy(out=acum[:].rearrange("p n h -> p (n h)"), in_=ps_seg[0][:, 0:256]), reads=[b_pseg[0]], writes=[b_ac])
    P.op("dve", lambda e: e.tensor_copy(out=tot[:].rearrange("p n h -> p (n h)"), in_=ps_seg[1][:, 0:256]), reads=[b_pseg[1]], writes=[b_ac])
    P.op("act", lambda e: e.activation(out=eacum[:], in_=acum[:], func=AF.Exp), reads=[b_ac], writes=[b_su])
    P.op("act", lambda e: e.activation(out=cdec[:], in_=tot[:], func=AF.Exp), reads=[b_ac], writes=[b_su])
    P.op("dve", lambda e: e.tensor_tensor(out=dlast[:], in0=tot[:], in1=acum[:], op=ALU.subtract), reads=[b_ac], writes=[b_su])
    P.op("act", lambda e: e.activation(out=dlast[:], in_=dlast[:], func=AF.Exp), reads=[b_su], writes=[b_su])

    xin = [C.sb([128, 8, TT + 3], F32, "xin") for _ in range(2)]
    b_xin = [Buf("xin0"), Buf("xin1")]
    acc = C.sb([128, 8, TT], F32, "acc")
    b_acc = [Buf(f"acc{c}") for c in range(8)]
    xc = C.sb([128, 8, TT], BF16, "xc")
    b_xc = Buf("xc")
    xsB = [C.sb([128, 768], BF16, "xsB") for _ in range(2)]
    b_xsB = [Buf("xsB0"), Buf("xsB1")]
    xsdt = [C.sb([128, 512], BF16, "xsdt") for _ in range(2)]
    b_xsdt = [Buf("xsdt0"), Buf("xsdt1")]
    xsdl = [C.sb([128, 512], BF16, "xsdl") for _ in range(2)]
    b_xsdl = [Buf("xsdl0"), Buf("xsdl1")]
    CBm = C.sb([128, 2, 128], F32, "CBm")
    b_CBm = Buf("CBm")
    NA = 4
    A_h = [C.sb([128, 128], F32, "A") for _ in range(NA)]
    b_A = [Buf(f"A{i}") for i in range(NA)]
    Lt = [C.sb([128, 128], F32, "Lt") for _ in range(NA)]
    b_Lt = [Buf(f"Lt{i}") for i in range(NA)]
    Mt = [C.sb([128, 128], BF16, "Mt") for _ in range(NA)]
    b_Mt = [Buf(f"Mt{i}") for i in range(NA)]
    S_f = C.sb([128, 2, 256], F32, "S_f")
    S_b = C.sb([128, 2, 256], BF16, "S_b")
    b_Sf = Buf("S_f")
    b_Sb = Buf("S_b")
    P.op("dve", lambda e: e.memset(S_f[:], 0.0), writes=[b_Sf])
    P.op("dve", lambda e: e.memset(S_b[:], 0.0), writes=[b_Sb])
    t1 = C.sb([128, 512], F32, "t1")
    b_t1 = Buf("t1")
    xsD = C.sb([128, 512], F32, "xsD")
    b_xsD = Buf("xsD")
    yb = C.sb([128, 512], F32, "y")
    b_y = Buf("y")
    z_sb = [C.sb([128, 512], F32, "z") for _ in range(2)]
    b_z = [Buf("z0"), Buf("z1")]
    ssq = C.sb([128, 2], F32, "ssq")
    b_ssq = Buf("ssq")
    junk = C.sb([128, 256], F32, "junk")
    b_junk = Buf("junk")
    ygn = C.sb([128, 512], BF16, "ygn")
    b_ygn = Buf("ygn")
    ygT = [C.sb([128, 4, TT], BF16, "ygT") for _ in range(2)]
    b_ygT = [Buf("ygT0"), Buf("ygT1")]
    ps_T = C.ps([128, 768], BF16, "psT")
    b_psT = Buf("psT")
    ps_cb = C.ps([128, 512], F32, "pscb")
    b_pscb = Buf("pscb")
    ps_yd = C.ps([128, 512], F32, "psyd")
    b_psyd = Buf("psyd")
    ps_yo = C.ps([128, 512], F32, "psyo")
    b_psyo = Buf("psyo")
    ps_st = C.ps([128, 512], F32, "psst")
    b_psst = Buf("psst")
    ps_yT = C.ps([128, 512], BF16, "psyT")
    b_psyT = Buf("psyT")
    xbc_v = K["xbcT"].rearrange("(c p) t -> p c t", p=128)
    mixed_v = K["mixedT"].rearrange("(c p) t -> p c t", p=128)
    ai = 0
    for it in range(NT):
        t0 = it * TT
        xi, bxi = xin[it % 2], b_xin[it % 2]
        if it == 0:
            P.op("dve", lambda e, xi=xi: e.memset(xi[:, :, 0:3], 0.0), writes=[bxi])
            P.dma("sp", xi[:, :, 3:TT + 3], xbc_v[:, :, 0:TT], writes=[bxi])
        else:
            P.dma("sp", xi[:], xbc_v[:, :, t0 - 3:t0 + TT], writes=[bxi])
        for c in range(8):
            P.op("dve", lambda e, c=c, xi=xi: e.tensor_scalar(out=acc[:, c, :], in0=xi[:, c, 3:TT + 3], scalar1=cw[:, c, 3:4], scalar2=cb[:, c:c + 1],
                                                           op0=ALU.mult, op1=ALU.add), reads=[bxi, b_par], writes=[b_acc[c]])
            for k in range(3):
                P.op("dve", lambda e, c=c, k=k, xi=xi: e.scalar_tensor_tensor(out=acc[:, c, :], in0=xi[:, c, k:TT + k], scalar=cw[:, c, k:k + 1], in1=acc[:, c, :],
                                                                           op0=ALU.mult, op1=ALU.add), reads=[bxi, b_par, b_acc[c]], writes=[b_acc[c]])
        P.op("act", lambda e: e.activation(out=xc[:], in_=acc[:], func=AF.Silu), reads=b_acc, writes=[b_xc])
        for q in range(4):
            n = it * 4 + q
            qs = slice(q * 128, (q + 1) * 128)
            xb_, bxb = xsB[n % 2], b_xsB[n % 2]
            xd, bxd = xsdt[n % 2], b_xsdt[n % 2]
            xl_, bxl = xsdl[n % 2], b_xsdl[n % 2]
            zb, bz = z_sb[n % 2], b_z[n % 2]
            P.dma("sp", zb[:], K["z_tm"][n * 128:(n + 1) * 128, :], writes=[bz])
            for cc in range(6):
                P.op("pe", lambda e, cc=cc, qs=qs: e.transpose(ps_T[:, cc * 128:(cc + 1) * 128], xc[:, cc, qs], ident_b[:]),
                     reads=[b_xc, bc], writes=[b_psT], inc=(cc == 5))
            P.op("act", lambda e, xb_=xb_: e.copy(out=xb_[:], in_=ps_T[:]), reads=[b_psT], writes=[bxb])
            P.op("dve", lambda e, xb_=xb_, xd=xd, n=n: e.tensor_tensor(out=xd[:].rearrange("p (h d) -> p h d", h=8),
                                                                       in0=xb_[:, 0:512].rearrange("p (h d) -> p h d", h=8),
                                                                       in1=dt[:, n, :].unsqueeze(2).to_broadcast([128, 8, 64]), op=ALU.mult),
                 reads=[bxb, b_dt], writes=[bxd])
            P.op("pool", lambda e, xl_=xl_, xd=xd, n=n: e.tensor_tensor(out=xl_[:].rearrange("p (h d) -> p h d", h=8),
                                                                        in0=xd[:].rearrange("p (h d) -> p h d", h=8),
                                                                        in1=dlast[:, n, :].unsqueeze(2).to_broadcast([128, 8, 64]), op=ALU.mult),
                 reads=[bxd, b_su], writes=[bxl])
            for g in range(2):
                P.op("pe", lambda e, g=g, qs=qs: e.matmul(ps_cb[:, g * 128:(g + 1) * 128], lhsT=xc[:, 4 + g, qs], rhs=xc[:, 6 + g, qs], start=True, stop=True),
                     reads=[b_xc], writes=[b_pscb], inc=(g == 1))
            P.op("dve", lambda e: e.tensor_tensor(out=CBm[:], in0=ps_cb[:, 0:256].rearrange("p (g i) -> p g i", g=2),
                                                  in1=U[:, :].unsqueeze(1).to_broadcast([128, 2, 128]), op=ALU.mult),
                 reads=[b_pscb, bc], writes=[b_CBm])
            for h in range(8):
                a_, ba_ = A_h[ai % NA], b_A[ai % NA]
                l_, bl_ = Lt[ai % NA], b_Lt[ai % NA]
                m_, bm_ = Mt[ai % NA], b_Mt[ai % NA]
                ai += 1
                pseg, bps = ps_seg[h // 4], b_pseg[h // 4]
                hs = slice((h % 4) * 128, (h % 4 + 1) * 128)
                P.op("pool", lambda e, a_=a_, n=n, h=h: e.tensor_scalar(out=a_[:], in0=SL[:], scalar1=adt[:, n, h:h + 1], scalar2=0.0, op0=ALU.mult, op1=ALU.add),
                     reads=[bc, b_su], writes=[ba_])
                P.op("pe", lambda e, a_=a_, pseg=pseg, hs=hs: e.matmul(pseg[:, hs], lhsT=a_[:], rhs=U[:], start=True, stop=True),
                     reads=[ba_, bc], writes=[bps])
                P.op("act", lambda e, l_=l_, pseg=pseg, hs=hs: e.activation(out=l_[:], in_=pseg[:, hs], func=AF.Exp), reads=[bps], writes=[bl_])
                P.op("dve", lambda e, m_=m_, l_=l_, h=h: e.tensor_tensor(out=m_[:], in0=CBm[:, h // 4, :], in1=l_[:], op=ALU.mult),
                     reads=[b_CBm, bl_], writes=[bm_])
                P.op("pe", lambda e, m_=m_, xd=xd, h=h: e.matmul(ps_yd[:, h * 64:(h + 1) * 64], lhsT=m_[:], rhs=xd[:, h * 64:(h + 1) * 64], start=True, stop=True),
                     reads=[bm_, bxd], writes=[b_psyd], inc=(h == 7))
            for g in range(2):
                P.op("pe", lambda e, g=g, qs=qs: e.matmul(ps_yo[:, g * 256:(g + 1) * 256], lhsT=xc[:, 6 + g, qs], rhs=S_b[:, g, :], start=True, stop=True),
                     reads=[b_xc, b_Sb], writes=[b_psyo], inc=(g == 1))
            P.op("dve", lambda e, n=n: e.tensor_tensor(out=t1[:].rearrange("p (h d) -> p h d", h=8), in0=ps_yo[:].rearrange("p (h d) -> p h d", h=8),
                                                       in1=eacum[:, n, :].unsqueeze(2).to_broadcast([128, 8, 64]), op=ALU.mult),
                 reads=[b_psyo, b_su], writes=[b_t1])
            P.op("pool", lambda e, xb_=xb_: e.tensor_tensor(out=xsD[:], in0=xb_[:, 0:512], in1=Dbc[:], op=ALU.mult), reads=[bxb, b_par], writes=[b_xsD])
            P.op("pool", lambda e: e.tensor_tensor(out=t1[:], in0=t1[:], in1=xsD[:], op=ALU.add), reads=[b_t1, b_xsD], writes=[b_t1])
            P.op("dve", lambda e: e.tensor_tensor(out=yb[:], in0=ps_yd[:], in1=t1[:], op=ALU.add), reads=[b_psyd, b_t1], writes=[b_y])
            for g in range(2):
                P.op("pe", lambda e, g=g, xb_=xb_, xl_=xl_: e.matmul(ps_st[:, g * 256:(g + 1) * 256], lhsT=xb_[:, 512 + g * 128:512 + (g + 1) * 128],
                                                                     rhs=xl_[:, g * 256:(g + 1) * 256], start=True, stop=True),
                     reads=[bxb, bxl], writes=[b_psst], inc=(g == 1))
            P.op("dve", lambda e, n=n: e.tensor_tensor(out=S_f[:].rearrange("p g (h d) -> p (g h) d", h=4), in0=S_f[:].rearrange("p g (h d) -> p (g h) d", h=4),
                                                       in1=cdec[:, n, :].unsqueeze(2).to_broadcast([128, 8, 64]), op=ALU.mult),
                 reads=[b_Sf, b_su], writes=[b_Sf])
            P.op("dve", lambda e: e.tensor_tensor(out=S_f[:].rearrange("p g d -> p (g d)"), in0=S_f[:].rearrange("p g d -> p (g d)"), in1=ps_st[:], op=ALU.add),
                 reads=[b_Sf, b_psst], writes=[b_Sf])
            P.op("act", lambda e: e.copy(out=S_b[:], in_=S_f[:]), reads=[b_Sf], writes=[b_Sb])
            P.op("act", lambda e, zb=zb: e.activation(out=zb[:], in_=zb[:], func=AF.Silu), reads=[bz], writes=[bz])
            P.op("dve", lambda e, zb=zb: e.tensor_tensor(out=yb[:], in0=yb[:], in1=zb[:], op=ALU.mult), reads=[b_y, bz], writes=[b_y])
            for g in range(2):
                P.op("act", lambda e, g=g: e.activation(out=junk[:], in_=yb[:, g * 256:(g + 1) * 256], func=AF.Square, accum_out=ssq[:, g:g + 1]),
                     reads=[b_y], writes=[b_junk, b_ssq])
            P.op("act", lambda e: e.activation(out=ssq[:], in_=ssq[:], func=AF.Sqrt, bias=eps_sb[:, 0:1], scale=1.0 / 256.0), reads=[b_ssq, bc], writes=[b_ssq])
            P.op("dve", lambda e: e.reciprocal(out=ssq[:], in_=ssq[:]), reads=[b_ssq], writes=[b_ssq])
            for g in range(2):
                P.op("act", lambda e, g=g: e.mul(out=ygn[:, g * 256:(g + 1) * 256], in_=yb[:, g * 256:(g + 1) * 256], mul=ssq[:, g:g + 1]),
                     reads=[b_y, b_ssq], writes=[b_ygn])
            for cc in range(4):
                P.op("pe", lambda e, cc=cc: e.transpose(ps_yT[:, cc * 128:(cc + 1) * 128], ygn[:, cc * 128:(cc + 1) * 128], ident_b[:]),
                     reads=[b_ygn, bc], writes=[b_psyT], inc=(cc == 3))
            yt, byt = ygT[it % 2], b_ygT[it % 2]
            P.op("dve", lambda e, yt=yt, qs=qs: e.tensor_copy(out=yt[:, :, qs], in_=ps_yT[:].rearrange("p (c t) -> p c t", c=4)), reads=[b_psyT], writes=[byt])
        P.dma("sp", mixed_v[:, 0:4, t0:t0 + TT], ygT[it % 2][:], reads=[b_ygT[it % 2]])
    P.barrier()
    C.release(m)


def lru_phase(P, C, K, l):
    TL = 1024
    NT = SEQ // TL
    m = C.mark()
    bc = K["b_const"]
    lcw = C.sb([128, 2, 4], F32, "lcw")
    lcb = C.sb([128, 2], F32, "lcb")
    ba = C.sb([128, 2], F32, "ba")
    bi = C.sb([128, 2], F32, "bi")
    cL = C.sb([128, 2], F32, "cL")
    Wa = C.sb([128, 2, 128], F32, "Wa")
    Wi = C.sb([128, 2, 128], F32, "Wi")
    b_par = Buf("lpar")
    b_cL = Buf("cL")
    P.dma("sp", lcw[:], K["lru_conv_w"][l][:, :, :], writes=[b_par])
    P.dma("sp", lcb[:], K["lru_conv_b"][l][:, :], writes=[b_par])
    P.dma("sp", ba[:], K["lru_ba"][l][:, :], writes=[b_par])
    P.dma("sp", bi[:], K["lru_bi"][l][:, :], writes=[b_par])
    P.dma("sp", cL[:], K["lru_lambda"][l][:, :], writes=[b_cL])
    P.dma("sp", Wa[:], K["lru_wa"][l][:, :, :], writes=[b_par])
    P.dma("sp", Wi[:], K["lru_wi"][l][:, :, :], writes=[b_par])
    P.op("dve", lambda e: e.tensor_scalar(out=cL[:], in0=cL[:], scalar1=-1.0, scalar2=None, op0=ALU.mult), reads=[b_cL], writes=[b_cL])
    softplus_small(P, C, cL[:], b_cL, [128, 2])
    P.op("dve", lambda e: e.tensor_scalar(out=cL[:], in0=cL[:], scalar1=-8.0, scalar2=None, op0=ALU.mult), reads=[b_cL], writes=[b_cL])
    xin = [C.sb([128, TL + 3], F32, "lxin") for _ in range(2)]
    b_xin = [Buf("lxin0"), Buf("lxin1")]
    gin = [C.sb([128, TL], F32, "gin") for _ in range(2)]
    b_gin = [Buf("gin0"), Buf("gin1")]
    xr = C.sb([128, TL], F32, "xr")
    b_xr = Buf("xr")
    r_ = C.sb([128, TL], F32, "r")
    b_r = Buf("r")
    i_ = C.sb([128, TL], F32, "i")
    b_i = Buf("i")
    a_ = C.sb([128, TL], F32, "a")
    b_a = Buf("a")
    om = C.sb([128, TL], F32, "om")
    b_om = Buf("om")
    hbuf = [[C.sb([128, TL], F32, "h") for _ in range(2)] for _ in range(2)]
    b_h = [[Buf("h00"), Buf("h01")], [Buf("h10"), Buf("h11")]]
    g2 = C.sb([128, TL], F32, "g2")
    b_g2 = Buf("g2")
    yo = [C.sb([128, TL], BF16, "ylru") for _ in range(2)]
    b_yo = [Buf("yo0"), Buf("yo1")]
    ps_r = [C.ps([128, 512], F32, "psr") for _ in range(2)]
    ps_i = [C.ps([128, 512], F32, "psi") for _ in range(2)]
    b_psr = [Buf("psr0"), Buf("psr1")]
    b_psi = [Buf("psi0"), Buf("psi1")]
    xlgl = K["xlglT"]
    mixed = K["mixedT"]
    k = 0
    for it in range(NT):
        t0 = it * TL
        for cc in range(2):
            xi, bxi = xin[k % 2], b_xin[k % 2]
            gi, bgi = gin[k % 2], b_gin[k % 2]
            yo_, byo = yo[k % 2], b_yo[k % 2]
            k += 1
            if it == 0:
                P.op("dve", lambda e, xi=xi: e.memset(xi[:, 0:3], 0.0), writes=[bxi])
                P.dma("sp", xi[:, 3:TL + 3], xlgl[cc * 128:(cc + 1) * 128, 0:TL], writes=[bxi])
            else:
                P.dma("sp", xi[:], xlgl[cc * 128:(cc + 1) * 128, t0 - 3:t0 + TL], writes=[bxi])
            P.dma("sp", gi[:], xlgl[256 + cc * 128:256 + (cc + 1) * 128, t0:t0 + TL], writes=[bgi])
            P.op("dve", lambda e, xi=xi, cc=cc: e.tensor_scalar(out=xr[:], in0=xi[:, 3:TL + 3], scalar1=lcw[:, cc, 3:4], scalar2=lcb[:, cc:cc + 1],
                                                             op0=ALU.mult, op1=ALU.add), reads=[bxi, b_par], writes=[b_xr])
            for kk in range(3):
                P.op("dve", lambda e, xi=xi, cc=cc, kk=kk: e.scalar_tensor_tensor(out=xr[:], in0=xi[:, kk:TL + kk], scalar=lcw[:, cc, kk:kk + 1], in1=xr[:],
                                                                               op0=ALU.mult, op1=ALU.add), reads=[bxi, b_par, b_xr], writes=[b_xr])
            for hf in range(2):
                hs = slice(hf * 512, (hf + 1) * 512)
                P.op("pe", lambda e, hf=hf, hs=hs, cc=cc: e.matmul(ps_r[hf][:], lhsT=Wa[:, cc, :], rhs=xr[:, hs], start=True, stop=True),
                     reads=[b_xr, b_par], writes=[b_psr[hf]])
                P.op("pe", lambda e, hf=hf, hs=hs, cc=cc: e.matmul(ps_i[hf][:], lhsT=Wi[:, cc, :], rhs=xr[:, hs], start=True, stop=True),
                     reads=[b_xr, b_par], writes=[b_psi[hf]])
                P.op("act", lambda e, hf=hf, hs=hs, cc=cc: e.activation(out=r_[:, hs], in_=ps_r[hf][:], func=AF.Sigmoid, bias=ba[:, cc:cc + 1]),
                     reads=[b_psr[hf], b_par], writes=[b_r])
                P.op("act", lambda e, hf=hf, hs=hs, cc=cc: e.activation(out=i_[:, hs], in_=ps_i[hf][:], func=AF.Sigmoid, bias=bi[:, cc:cc + 1]),
                     reads=[b_psi[hf], b_par], writes=[b_i])
            P.op("act", lambda e, cc=cc: e.activation(out=a_[:], in_=r_[:], func=AF.Exp, scale=cL[:, cc:cc + 1]), reads=[b_r, b_cL], writes=[b_a])
            P.op("dve", lambda e: e.tensor_tensor(out=om[:], in0=a_[:], in1=a_[:], op=ALU.mult), reads=[b_a], writes=[b_om])
            P.op("dve", lambda e: e.tensor_scalar(out=om[:], in0=om[:], scalar1=-1.0, scalar2=1.0, op0=ALU.mult, op1=ALU.add), reads=[b_om], writes=[b_om])
            P.op("act", lambda e: e.activation(out=om[:], in_=om[:], func=AF.Sqrt), reads=[b_om], writes=[b_om])
            P.op("pool", lambda e: e.tensor_tensor(out=i_[:], in0=i_[:], in1=xr[:], op=ALU.mult), reads=[b_i, b_xr], writes=[b_i])
            P.op("dve", lambda e: e.tensor_tensor(out=om[:], in0=om[:], in1=i_[:], op=ALU.mult), reads=[b_om, b_i], writes=[b_om])
            hcur, bh = hbuf[cc][it % 2], b_h[cc][it % 2]
            hprev, bhp = hbuf[cc][(it + 1) % 2], b_h[cc][(it + 1) % 2]
            if it == 0:
                P.op("dve", lambda e, hcur=hcur: e.tensor_tensor_scan(out=hcur[:], data0=a_[:], data1=om[:], initial=0.0, op0=ALU.mult, op1=ALU.add),
                     reads=[b_a, b_om], writes=[bh])
            else:
                P.op("dve", lambda e, hcur=hcur, hprev=hprev: e.tensor_tensor_scan(out=hcur[:], data0=a_[:], data1=om[:], initial=hprev[:, TL - 1:TL],
                                                                                 op0=ALU.mult, op1=ALU.add),
                     reads=[b_a, b_om, bhp], writes=[bh])
            P.op("pool", lambda e, gi=gi: e.tensor_tensor(out=g2[:], in0=gi[:], in1=gi[:], op=ALU.mult), reads=[bgi], writes=[b_g2])
            P.op("pool", lambda e: e.tensor_scalar(out=g2[:], in0=g2[:], scalar1=0.044715, scalar2=1.0, op0=ALU.mult, op1=ALU.add), reads=[b_g2], writes=[b_g2])
            P.op("pool", lambda e, gi=gi: e.tensor_tensor(out=g2[:], in0=g2[:], in1=gi[:], op=ALU.mult), reads=[b_g2, bgi], writes=[b_g2])
            P.op("act", lambda e: e.activation(out=g2[:], in_=g2[:], func=AF.Sigmoid, scale=1.5957691216057308), reads=[b_g2], writes=[b_g2])
            P.op("pool", lambda e, gi=gi: e.tensor_tensor(out=g2[:], in0=g2[:], in1=gi[:], op=ALU.mult), reads=[b_g2, bgi], writes=[b_g2])
            P.op("dve", lambda e, hcur=hcur, yo_=yo_: e.tensor_tensor(out=yo_[:], in0=hcur[:], in1=g2[:], op=ALU.mult), reads=[bh, b_g2], writes=[byo])
            P.dma("sp", mixed[768 + cc * 128:768 + (cc + 1) * 128, t0:t0 + TL], yo_[:], reads=[byo])
    P.barrier()
    C.release(m)

NBIS = 18
BIG = 1.0e30


def rope_tm(P, eng, x3, bx, cosn, sinn, nh, tmp, bt, btab):
    cb = cosn.unsqueeze(1).to_broadcast([128, nh, 8])
    sb_ = sinn.unsqueeze(1).to_broadcast([128, nh, 8])
    x1 = x3[:, :, 0:8]
    x2 = x3[:, :, 8:16]
    t1, t2, t3, t4 = tmp[:, 0, 0:nh, :], tmp[:, 1, 0:nh, :], tmp[:, 2, 0:nh, :], tmp[:, 3, 0:nh, :]
    P.op(eng, lambda e: e.tensor_tensor(out=t1, in0=x1, in1=cb, op=ALU.mult), reads=[bx, btab], writes=[bt])
    P.op(eng, lambda e: e.tensor_tensor(out=t2, in0=x2, in1=sb_, op=ALU.mult), reads=[bx, btab], writes=[bt])
    P.op(eng, lambda e: e.tensor_tensor(out=t3, in0=x1, in1=sb_, op=ALU.mult), reads=[bx, btab], writes=[bt])
    P.op(eng, lambda e: e.tensor_tensor(out=t4, in0=x2, in1=cb, op=ALU.mult), reads=[bx, btab], writes=[bt])
    P.op(eng, lambda e: e.tensor_tensor(out=x1, in0=t1, in1=t2, op=ALU.subtract), reads=[bt], writes=[bx])
    P.op(eng, lambda e: e.tensor_tensor(out=x2, in0=t3, in1=t4, op=ALU.add), reads=[bt], writes=[bx])


def dsa_phase(P, C, K, l):
    m = C.mark()
    bc = K["b_const"]
    ident_b, ident_f, eps_sb = K["ident_b"], K["ident_f"], K["eps_sb"]
    NB = SEQ // 128
    gq = C.sb([128, 2], F32, "gq")
    gkv = C.sb([128, 1], F32, "gkv")
    b_g = Buf("gqkv")
    P.dma("sp", gq[:], K["cq_norm"][l][:, :], writes=[b_g])
    P.dma("sp", gkv[:], K["ckv_norm"][l][:, :], writes=[b_g])
    Wq = C.sb([128, 2, 256], BF16, "Wq")
    Wqi = C.sb([128, 2, 256], BF16, "Wqi")
    Wkv = C.sb([128, 512], BF16, "Wkv")
    b_W = Buf("W")
    load_cast_scaled(P, C, Wq, b_W, K["w_uq"][l][:, :, :], [128, 2, 256], gq, b_g, nchunk=2)
    load_cast_scaled(P, C, Wqi, b_W, K["w_qidx"][l][:, :, :], [128, 2, 256], gq, b_g, nchunk=2)
    load_cast_scaled(P, C, Wkv[:], b_W, K["w_ukv"][l][:, :], [128, 512], gkv, b_g)
    qg = C.sb([128, 64], F32, "qg")
    kg = C.sb([128, 64], F32, "kg")
    kig = C.sb([128, 64], F32, "kig")
    cos = C.sb([128, NB, 8], F32, "cos")
    sin = C.sb([128, NB, 8], F32, "sin")
    CM = C.sb([128, 128], F32, "CM")
    rdiv = C.sb([128, 3], F32, "rdiv")
    pw2 = C.sb([128, NBIS], F32, "pw2")
    b_tab = Buf("tab")
    P.dma("sp", qg[:], K["q_norm"][l][:, :], writes=[b_tab])
    P.dma("sp", kg[:], K["k_norm"][l][:, :], writes=[b_tab])
    P.dma("sp", kig[:], K["kidx_norm"][l][:, :], writes=[b_tab])
    P.dma("sp", cos[:], K["c_cos"][:, :, :], writes=[b_tab])
    P.dma("sp", sin[:], K["c_sin"][:, :, :], writes=[b_tab])
    P.dma("sp", CM[:], K["c_cmask"][:, :], writes=[b_tab])
    P.dma("sp", rdiv[:], K["c_rdiv"][:, :], writes=[b_tab])
    P.dma("sp", pw2[:], K["c_pw2"][:, :], writes=[b_tab])
    cqT = C.sb([128, 3, SEQ], BF16, "cqT")
    b_cqT = Buf("cqT")
    cq_v = K["cqkvT"].rearrange("(c p) t -> p c t", p=128)
    for i in range(4):
        P.dma("sp", cqT[:, :, i * 1024:(i + 1) * 1024], cq_v[:, :, i * 1024:(i + 1) * 1024], writes=[b_cqT])
    kT = C.sb([128, 2, SEQ], BF16, "kT")
    b_kT = Buf("kT")
    kiT = C.sb([128, SEQ], F32, "kiT")
    b_kiT = Buf("kiT")
    vaug = C.sb([128, NB, 4, 65], BF16, "vaug")
    b_v = Buf("vaug")
    P.op("pool", lambda e: e.memset(vaug[:], 1.0), writes=[b_v])
    score = C.sb([128, SEQ], F32, "score")
    b_sc = Buf("score")
    mask = C.sb([128, SEQ], BF16, "mask")
    b_mask = Buf("mask")
    junk = C.sb([128, SEQ], BF16, "junkb")
    b_junk = Buf("junkb")
    att = [C.sb([128, 460], F32, "att") for _ in range(2)]
    b_att = [Buf("att0"), Buf("att1")]
    sq = C.sb([128, 256], F32, "sqj")
    b_sq = Buf("sqj")
    st3 = C.sb([128, 3], F32, "st3")
    b_st3 = Buf("st3")
    qs_ = C.sb([128, 4, 64], F32, "qs")
    b_qs = Buf("qs")
    ks_ = C.sb([128, 4, 64], F32, "ks")
    b_ks = Buf("ks")
    qis = C.sb([128, 4, 64], F32, "qis")
    b_qis = Buf("qis")
    ki2 = C.sb([128, 2, 64], F32, "ki2")
    b_ki2 = Buf("ki2")
    sq4 = C.sb([128, 4, 64], F32, "sq4")
    b_sq4 = Buf("sq4")
    r4 = C.sb([128, 8], F32, "r4")
    b_r4 = Buf("r4")
    rtmp = C.sb([128, 4, 4, 8], F32, "rtmp")
    b_rtmp = Buf("rtmp")
    rtmp2 = C.sb([128, 4, 4, 8], F32, "rtmp2")
    b_rtmp2 = Buf("rtmp2")
    wsc = C.sb([128, 4], F32, "wsc")
    lo4 = C.sb([128, 4], F32, "lo4")
    hi4 = C.sb([128, 4], F32, "hi4")
    b_w4 = Buf("w4")
    q_b = C.sb([128, 256], BF16, "q_b")
    b_qb = Buf("q_b")
    k_b = C.sb([128, 256], BF16, "k_b")
    b_kb = Buf("k_b")
    qT = [C.sb([128, 4, 128], BF16, "qTz") for _ in range(2)]
    b_qT = [Buf("qT0"), Buf("qT1")]
    for i in range(2):
        P.op("pool", lambda e, i=i: e.memset(qT[i][:], 0.0), writes=[b_qT[i]])
    qiT = [C.sb([128, 2, 128], F32, "qiT") for _ in range(2)]
    b_qiT = [Buf("qiT0"), Buf("qiT1")]
    clp = [C.sb([128, 512], F32, "clp") for _ in range(2)]
    b_clp = [Buf("clp0"), Buf("clp1")]
    lo = C.sb([128, 1], F32, "lo")
    Wd = C.sb([128, 1], F32, "Wd")
    Wk = C.sb([128, NBIS], F32, "Wk")
    cnt = C.sb([128, 1], F32, "cnt")
    dlt = C.sb([128, 1], F32, "dlt")
    mid = C.sb([128, 1], F32, "mid")
    b_bis = Buf("bis")
    b_cnt = Buf("cnt")
    b_mid = Buf("mid")
    b_dlt = Buf("dlt")
    pT = [C.sb([128, 4, 128], BF16, "pT") for _ in range(3)]
    b_pT = [Buf(f"pT{i}") for i in range(3)]
    rden = C.sb([128, 4], F32, "rden")
    b_rden = Buf("rden")
    yat = C.sb([128, 256], BF16, "yat")
    b_yat = Buf("yat")
    yaT = [C.sb([128, 2, 128], BF16, "yaT") for _ in range(2)]
    b_yaT = [Buf("yaT0"), Buf("yaT1")]
    ps_pr = [C.ps([128, 512], F32, "pspr") for _ in range(2)]
    b_pspr = [Buf("pspr0"), Buf("pspr1")]
    ps_sc = [C.ps([128, 512], F32, "pssc") for _ in range(2)]
    b_pssc = [Buf("pssc0"), Buf("pssc1")]
    ps_tr = C.ps([128, 512], BF16, "pstr")
    b_pstr = Buf("pstr")
    ps_trf = C.ps([128, 512], F32, "pstrf")
    b_pstrf = Buf("pstrf")
    ps_S = [C.ps([128, 512], F32, "psS") for _ in range(1)]
    b_psS = [Buf("psS0")]
    ps_o = C.ps([128, 512], F32, "pso")
    b_pso = Buf("pso")
    ps_mT = ps_tr
    mixed_v = K["mixedT"].rearrange("(c p) t -> p c t", p=128)
    sci = 0
    pti = 0
    dbg = K.get('dbg', {})
    stage = dbg.get('dsa_stage', 9)
    for n in range(dbg.get('dsa_nb', NB)):
        ns = slice(n * 128, (n + 1) * 128)
        at, bat = att[n % 2], b_att[n % 2]
        P.dma("sp", at[:], K["att_tm"][n * 128:(n + 1) * 128, :], writes=[bat])
        cosn, sinn = cos[:, n, :], sin[:, n, :]
        for j, (a0, a1) in enumerate(((8, 264), (264, 392), (392, 456))):
            P.op("act", lambda e, a0=a0, a1=a1, j=j: e.activation(out=sq[:, 0:a1 - a0], in_=at[:, a0:a1], func=AF.Square, accum_out=st3[:, j:j + 1]),
                 reads=[bat], writes=[b_sq, b_st3])
        P.op("dve", lambda e: e.tensor_tensor(out=st3[:], in0=st3[:], in1=rdiv[:], op=ALU.mult), reads=[b_st3, b_tab], writes=[b_st3])
        P.op("act", lambda e: e.activation(out=st3[:], in_=st3[:], func=AF.Sqrt, bias=eps_sb[:, 0:1]), reads=[b_st3, bc], writes=[b_st3])
        P.op("dve", lambda e: e.reciprocal(out=st3[:], in_=st3[:]), reads=[b_st3], writes=[b_st3])
        pq, bpq = ps_pr[0], b_pspr[0]
        pk, bpk = ps_pr[1], b_pspr[1]
        for kc in range(2):
            P.op("pe", lambda e, kc=kc: e.matmul(pq[:, 0:256], lhsT=cqT[:, kc, ns], rhs=Wq[:, kc, :], start=(kc == 0), stop=(kc == 1)),
                 reads=[b_cqT, b_W], writes=[bpq], inc=False)
        for kc in range(2):
            P.op("pe", lambda e, kc=kc: e.matmul(pq[:, 256:512], lhsT=cqT[:, kc, ns], rhs=Wqi[:, kc, :], start=False, stop=(kc == 1), skip_group_check=True),
                 reads=[b_cqT, b_W], writes=[bpq], inc=(kc == 1))
        P.op("pe", lambda e: e.matmul(pk[:, 0:512], lhsT=cqT[:, 2, ns], rhs=Wkv[:], start=True, stop=True), reads=[b_cqT, b_W], writes=[bpk])
        P.op("dve", lambda e: e.tensor_scalar(out=qs_[:].rearrange("p h d -> p (h d)"), in0=pq[:, 0:256], scalar1=st3[:, 0:1], scalar2=None, op0=ALU.mult),
             reads=[bpq, b_st3], writes=[b_qs])
        P.op("dve", lambda e: e.tensor_scalar(out=qis[:].rearrange("p h d -> p (h d)"), in0=pq[:, 256:512], scalar1=st3[:, 0:1], scalar2=None, op0=ALU.mult),
             reads=[bpq, b_st3], writes=[b_qis])
        P.op("dve", lambda e: e.tensor_scalar(out=ks_[:], in0=pk[:, 0:512].rearrange("p (h d) -> p h d", h=4)[:, :, 0:64], scalar1=st3[:, 1:2], scalar2=None, op0=ALU.mult),
             reads=[bpk, b_st3], writes=[b_ks])
        P.op("act", lambda e, n=n: e.mul(out=vaug[:, n, :, 0:64], in_=pk[:, 0:512].rearrange("p (h d) -> p h d", h=4)[:, :, 64:128], mul=st3[:, 1:2]),
             reads=[bpk, b_st3], writes=[b_v])
        for (x_, bx_, c0, gain) in ((qs_, b_qs, 0, qg), (ks_, b_ks, 4, kg)):
            P.op("pool", lambda e, x_=x_: e.tensor_tensor(out=sq4[:], in0=x_[:], in1=x_[:], op=ALU.mult), reads=[bx_], writes=[b_sq4])
            P.op("dve", lambda e, c0=c0: e.tensor_reduce(out=r4[:, c0:c0 + 4], in_=sq4[:], axis=AX.X, op=ALU.add), reads=[b_sq4], writes=[b_r4])
            P.op("act", lambda e, c0=c0: e.activation(out=r4[:, c0:c0 + 4], in_=r4[:, c0:c0 + 4], func=AF.Sqrt, bias=eps_sb[:, 0:1], scale=1.0 / 64.0),
                 reads=[b_r4, bc], writes=[b_r4])
            P.op("dve", lambda e, c0=c0: e.reciprocal(out=r4[:, c0:c0 + 4], in_=r4[:, c0:c0 + 4]), reads=[b_r4], writes=[b_r4])
            P.op("dve", lambda e, x_=x_, c0=c0: e.tensor_tensor(out=x_[:], in0=x_[:], in1=r4[:, c0:c0 + 4].unsqueeze(2).to_broadcast([128, 4, 64]), op=ALU.mult),
                 reads=[bx_, b_r4], writes=[bx_])
            P.op("pool", lambda e, x_=x_, gain=gain: e.tensor_tensor(out=x_[:], in0=x_[:], in1=gain[:, :].unsqueeze(1).to_broadcast([128, 4, 64]), op=ALU.mult),
                 reads=[bx_, b_tab], writes=[bx_])
        rope_tm(P, "dve", qs_[:], b_qs, cosn, sinn, 4, rtmp, b_rtmp, b_tab)
        rope_tm(P, "pool", ks_[:], b_ks, cosn, sinn, 4, rtmp2, b_rtmp2, b_tab)
        P.op("act", lambda e: e.copy(out=q_b[:], in_=qs_[:].rearrange("p h d -> p (h d)")), reads=[b_qs], writes=[b_qb])
        P.op("act", lambda e: e.copy(out=k_b[:], in_=ks_[:].rearrange("p h d -> p (h d)")), reads=[b_ks], writes=[b_kb])
        qT_, bqT_ = qT[n % 2], b_qT[n % 2]
        for pr in range(2):
            P.op("pe", lambda e, pr=pr: e.transpose(ps_tr[:, pr * 128:(pr + 1) * 128], q_b[:, pr * 128:(pr + 1) * 128], ident_b[:]),
                 reads=[b_qb, bc], writes=[b_pstr], inc=False)
        for pr in range(2):
            P.op("pe", lambda e, pr=pr: e.transpose(ps_tr[:, 256 + pr * 128:256 + (pr + 1) * 128], k_b[:, pr * 128:(pr + 1) * 128], ident_b[:]),
                 reads=[b_kb, bc], writes=[b_pstr], inc=(pr == 1))
        for h in range(4):
            hp = slice((h % 2) * 64, (h % 2) * 64 + 64)
            if h % 2 == 0:
                P.op("dve", lambda e, qT_=qT_, h=h, hp=hp: e.tensor_copy(out=qT_[hp, h, :], in_=ps_tr[hp, (h // 2) * 128:(h // 2 + 1) * 128]), reads=[b_pstr], writes=[bqT_])
            else:
                P.op("act", lambda e, qT_=qT_, h=h, hp=hp: e.copy(out=qT_[hp, h, :], in_=ps_tr[hp, (h // 2) * 128:(h // 2 + 1) * 128]), reads=[b_pstr], writes=[bqT_])
        P.op("dve", lambda e: e.tensor_copy(out=kT[:, :, ns], in_=ps_tr[:, 256:512].rearrange("p (c t) -> p c t", c=2)), reads=[b_pstr], writes=[b_kT])
        P.op("dve", lambda e: e.tensor_scalar(out=ki2[:, 0, :], in0=at[:, 392:456], scalar1=st3[:, 2:3], scalar2=None, op0=ALU.mult), reads=[bat, b_st3], writes=[b_ki2])
        P.op("dve", lambda e: e.tensor_tensor(out=ki2[:, 0, :], in0=ki2[:, 0, :], in1=kig[:], op=ALU.mult), reads=[b_ki2, b_tab], writes=[b_ki2])
        rope_tm(P, "dve", ki2[:, 0:1, :], b_ki2, cosn, sinn, 1, rtmp, b_rtmp, b_tab)
        P.op("dve", lambda e: e.tensor_copy(out=ki2[:, 1, :], in_=ki2[:, 0, :]), reads=[b_ki2], writes=[b_ki2])
        rope_tm(P, "pool", qis[:], b_qis, cosn, sinn, 4, rtmp2, b_rtmp2, b_tab)
        P.op("dve", lambda e: e.tensor_scalar(out=wsc[:], in0=at[:, 456:460], scalar1=0.5 * 0.125, scalar2=None, op0=ALU.mult), reads=[bat], writes=[b_w4])
        P.op("dve", lambda e: e.tensor_scalar(out=lo4[:], in0=wsc[:], scalar1=0.0, scalar2=-BIG, op0=ALU.is_lt, op1=ALU.mult), reads=[b_w4], writes=[b_w4])
        P.op("dve", lambda e: e.tensor_scalar(out=hi4[:], in0=wsc[:], scalar1=0.0, scalar2=BIG, op0=ALU.is_ge, op1=ALU.mult), reads=[b_w4], writes=[b_w4])
        P.op("dve", lambda e: e.tensor_tensor(out=qis[:], in0=qis[:], in1=wsc[:, :].unsqueeze(2).to_broadcast([128, 4, 64]), op=ALU.mult),
             reads=[b_qis, b_w4], writes=[b_qis])
        qiT_, bqiT_ = qiT[n % 2], b_qiT[n % 2]
        for pr in range(2):
            P.op("pe", lambda e, pr=pr: e.transpose(ps_trf[:, pr * 128:(pr + 1) * 128], qis[:, 2 * pr:2 * pr + 2, :].rearrange("p h d -> p (h d)"), ident_f[:]),
                 reads=[b_qis, bc], writes=[b_pstrf], inc=False)
        P.op("pe", lambda e: e.transpose(ps_trf[:, 256:384], ki2[:].rearrange("p h d -> p (h d)"), ident_f[:]), reads=[b_ki2, bc], writes=[b_pstrf])
        P.op("act", lambda e, qiT_=qiT_: e.copy(out=qiT_[:], in_=ps_trf[:, 0:256].rearrange("p (c t) -> p c t", c=2)), reads=[b_pstrf], writes=[bqiT_])
        P.op("act", lambda e: e.copy(out=kiT[:, ns], in_=ps_trf[:, 256:384]), reads=[b_pstrf], writes=[b_kiT])
        nk = (n + 1) * 128
        if stage < 2:
            continue
        for g0 in range(0, nk, 512):
            wd = min(512, nk - g0)
            for h in range(4):
                psc, bpsc = ps_sc[sci % 2], b_pssc[sci % 2]
                cl, bcl = clp[sci % 2], b_clp[sci % 2]
                sci += 1
                hp = slice((h % 2) * 64, (h % 2) * 64 + 64)
                P.op("pe", lambda e, psc=psc, h=h, hp=hp, g0=g0, wd=wd, qiT_=qiT_: e.matmul(psc[:, 0:wd], lhsT=qiT_[hp, h // 2, :], rhs=kiT[hp, g0:g0 + wd],
                                                                                          start=True, stop=True),
                     reads=[bqiT_, b_kiT], writes=[bpsc])
                if h == 0:
                    P.op("dve", lambda e, psc=psc, h=h, g0=g0, wd=wd: e.tensor_scalar(out=score[:, g0:g0 + wd], in0=psc[:, 0:wd], scalar1=lo4[:, h:h + 1], scalar2=hi4[:, h:h + 1],
                                                                                     op0=ALU.max, op1=ALU.min), reads=[bpsc, b_w4], writes=[b_sc])
                else:
                    P.op("dve", lambda e, psc=psc, h=h, cl=cl, wd=wd: e.tensor_scalar(out=cl[:, 0:wd], in0=psc[:, 0:wd], scalar1=lo4[:, h:h + 1], scalar2=hi4[:, h:h + 1],
                                                                                     op0=ALU.max, op1=ALU.min), reads=[bpsc, b_w4], writes=[bcl])
                    P.op("pool", lambda e, cl=cl, g0=g0, wd=wd: e.tensor_tensor(out=score[:, g0:g0 + wd], in0=score[:, g0:g0 + wd], in1=cl[:, 0:wd], op=ALU.add),
                         reads=[bcl, b_sc], writes=[b_sc])
        P.op("dve", lambda e: e.tensor_tensor(out=score[:, ns], in0=score[:, ns], in1=CM[:], op=ALU.add), reads=[b_sc, b_tab], writes=[b_sc])
        if stage < 3:
            continue
        if n < 2:
            P.op("dve", lambda e: e.memset(lo[:], -1.0e29), writes=[b_bis])
        else:
            P.op("dve", lambda e: e.tensor_reduce(out=lo[:], in_=score[:, 0:n * 128], axis=AX.X, op=ALU.min), reads=[b_sc], writes=[b_bis])
            P.op("dve", lambda e: e.tensor_reduce(out=Wd[:], in_=score[:, 0:nk], axis=AX.X, op=ALU.max), reads=[b_sc], writes=[b_bis])
            P.op("dve", lambda e: e.tensor_scalar(out=Wd[:], in0=Wd[:], scalar1=lo[:, 0:1], scalar2=1.0e-6, op0=ALU.subtract, op1=ALU.add), reads=[b_bis], writes=[b_bis])
            P.op("dve", lambda e: e.tensor_scalar(out=Wk[:], in0=pw2[:], scalar1=Wd[:, 0:1], scalar2=None, op0=ALU.mult), reads=[b_bis, b_tab], writes=[b_bis])
            P.op("dve", lambda e: e.tensor_tensor(out=mid[:], in0=lo[:], in1=Wk[:, 0:1], op=ALU.add), reads=[b_bis], writes=[b_mid])
            for it in range(NBIS):
                P.op("dve", lambda e: e.tensor_scalar(out=junk[:, 0:nk], in0=score[:, 0:nk], scalar1=mid[:, 0:1], scalar2=0.0,
                                                      op0=ALU.is_ge, op1=ALU.add, accum_out=cnt[:, 0:1]), reads=[b_sc, b_mid], writes=[b_junk, b_cnt])
                P.op("dve", lambda e: e.tensor_scalar(out=dlt[:], in0=cnt[:], scalar1=255.5, scalar2=0.5, op0=ALU.is_ge, op1=ALU.subtract),
                     reads=[b_cnt], writes=[b_dlt])
                if it < NBIS - 1:
                    P.op("dve", lambda e, it=it: e.scalar_tensor_tensor(out=mid[:], in0=dlt[:], scalar=Wk[:, it:it + 1], in1=mid[:], op0=ALU.mult, op1=ALU.add),
                         reads=[b_dlt, b_bis, b_mid], writes=[b_mid])
                else:
                    P.op("dve", lambda e: e.tensor_scalar(out=dlt[:], in0=dlt[:], scalar1=0.5, scalar2=None, op0=ALU.subtract), reads=[b_dlt], writes=[b_dlt])
                    P.op("dve", lambda e, it=it: e.scalar_tensor_tensor(out=lo[:], in0=dlt[:], scalar=Wk[:, it:it + 1], in1=mid[:], op0=ALU.mult, op1=ALU.add),
                         reads=[b_dlt, b_bis, b_mid], writes=[b_bis])
        P.op("dve", lambda e: e.tensor_scalar(out=mask[:, 0:nk], in0=score[:, 0:nk], scalar1=lo[:, 0:1], scalar2=None, op0=ALU.is_ge), reads=[b_sc, b_bis], writes=[b_mask])
        if stage < 4:
            continue
        for kb in range(n + 1):
            ks = slice(kb * 128, (kb + 1) * 128)
            pS, bpS = ps_S[0], b_psS[0]
            p_, bp_ = pT[pti % 3], b_pT[pti % 3]
            pti += 1
            P.op("pe", lambda e, ks=ks: e.transpose(ps_mT[:, 256:384], mask[:, ks], ident_b[:]), reads=[b_mask, bc], writes=[b_pstr])
            if dbg.get('dsa_sub', 9) < 1:
                continue
            for h in dbg.get('heads', range(4)):
                hp = slice((h % 2) * 64, (h % 2) * 64 + 64)
                P.op("pe", lambda e, h=h, hp=hp, ks=ks, qT_=qT_: e.matmul(pS[:, h * 128:(h + 1) * 128], lhsT=kT[:, h // 2, ks], rhs=qT_[:, h, :], start=True, stop=True),
                     reads=[b_kT, bqT_], writes=[bpS], inc=(h == 3 or 'heads' in dbg))
            P.op("act", lambda e, p_=p_: e.activation(out=p_[:].rearrange("p h q -> p (h q)"), in_=pS[:, 0:512], func=AF.Exp, scale=0.125), reads=[bpS], writes=[bp_])
            if dbg.get('dsa_sub', 9) < 2:
                continue
            P.op("dve", lambda e, p_=p_: e.tensor_tensor(out=p_[:], in0=p_[:], in1=ps_mT[:, 256:384].unsqueeze(1).to_broadcast([128, 4, 128]), op=ALU.mult),
                 reads=[bp_, b_pstr], writes=[bp_])
            if dbg.get('dsa_sub', 9) < 3:
                continue
            for h in range(4):
                P.op("pe", lambda e, h=h, kb=kb, p_=p_, n=n: e.matmul(ps_o[:, h * 65:(h + 1) * 65], lhsT=p_[:, h, :], rhs=vaug[:, kb, h, :],
                                                                     start=(kb == 0 and h == 0), stop=(kb == n and h == 3), skip_group_check=True),
                     reads=[bp_, b_v], writes=[b_pso], inc=(h == 3))
        if dbg.get('dsa_sub', 9) < 4:
            continue
        po3 = ps_o[:, 0:260].rearrange("p (h d) -> p h d", h=4)
        P.op("dve", lambda e: e.reciprocal(out=rden[:].unsqueeze(2), in_=po3[:, :, 64:65]), reads=[b_pso], writes=[b_rden])
        P.op("dve", lambda e: e.tensor_tensor(out=yat[:].rearrange("p (h d) -> p h d", h=4), in0=po3[:, :, 0:64], in1=rden[:, :].unsqueeze(2).to_broadcast([128, 4, 64]), op=ALU.mult),
             reads=[b_pso, b_rden], writes=[b_yat])
        for pr in range(2):
            P.op("pe", lambda e, pr=pr: e.transpose(ps_tr[:, pr * 128:(pr + 1) * 128], yat[:, pr * 128:(pr + 1) * 128], ident_b[:]),
                 reads=[b_yat, bc], writes=[b_pstr], inc=(pr == 1))
        ya_, bya_ = yaT[n % 2], b_yaT[n % 2]
        P.op("act", lambda e, ya_=ya_: e.copy(out=ya_[:], in_=ps_tr[:, 0:256].rearrange("p (c t) -> p c t", c=2)), reads=[b_pstr], writes=[bya_])
        P.dma("sp", mixed_v[:, 4:6, ns], ya_[:], reads=[bya_])
    P.barrier()
    C.release(m)


SCRATCH = {
    "xT": ([8, 128, SEQ], F32),
    "z_tm": ([SEQ, 512], F32),
    "att_tm": ([SEQ, 460], F32),
    "xbcT": ([1024, SEQ], F32),
    "cqkvT": ([384, SEQ], BF16),
    "xlglT": ([512, SEQ], F32),
    "mixedT": ([1024, SEQ], BF16),
    "w_out_s": ([128, 8, D_MODEL], BF16),
}

PARAMS = {
    "ffn1_w13": [DEPTH, 128, 8, 2 * D_FF], "ffn1_w2": [DEPTH, 128, 22, D_MODEL], "norm_ffn1": [DEPTH, 128, 8],
    "ffn2_w13": [DEPTH, 128, 8, 2 * D_FF], "ffn2_w2": [DEPTH, 128, 22, D_MODEL], "norm_ffn2": [DEPTH, 128, 8],
    "norm_mix": [DEPTH, 128, 8], "w_in": [DEPTH, 128, 8, IN_COLS], "w_out": [DEPTH, 128, 8, D_MODEL],
    "ssd_conv_w": [DEPTH, 128, 8, 4], "ssd_conv_b": [DEPTH, 128, 8], "ssd_dt_bias": [DEPTH, 128, 8], "ssd_a_log": [DEPTH, 128, 8],
    "ssd_d": [DEPTH, 128, 512], "ssd_norm": [DEPTH, 128, 4],
    "cq_norm": [DEPTH, 128, 2], "ckv_norm": [DEPTH, 128, 1], "w_uq": [DEPTH, 128, 2, 256], "w_qidx": [DEPTH, 128, 2, 256], "w_ukv": [DEPTH, 128, 512],
    "q_norm": [DEPTH, 128, 64], "k_norm": [DEPTH, 128, 64], "kidx_norm": [DEPTH, 128, 64],
    "lru_conv_w": [DEPTH, 128, 2, 4], "lru_conv_b": [DEPTH, 128, 2], "lru_ba": [DEPTH, 128, 2], "lru_bi": [DEPTH, 128, 2], "lru_lambda": [DEPTH, 128, 2],
    "lru_wa": [DEPTH, 128, 2, 128], "lru_wi": [DEPTH, 128, 2, 128],
    "c_ident_f": [128, 128], "c_U": [128, 128], "c_SL": [128, 128], "c_cos": [128, 32, 8], "c_sin": [128, 32, 8], "c_cmask": [128, 128],
    "c_rdiv": [128, 3], "c_pw2": [128, NBIS],
}


def build_program(debug=None):
    debug = debug or {}
    nc = bass.Bass("TRN2", target_bir_lowering=False)
    P = Prog(nc)
    C = Ctx(nc)
    K = {'dbg': debug}
    x = nc.dram_tensor("x", [SEQ, D_MODEL], F32, kind="ExternalInput").ap()
    y = nc.dram_tensor("y", [SEQ, D_MODEL], F32, kind="ExternalOutput").ap()
    for name, shape in PARAMS.items():
        K[name] = nc.dram_tensor(name, list(shape), F32, kind="ExternalInput").ap()
    for name, (shape, dt) in SCRATCH.items():
        kind = "Internal"
        if name in debug.get("ext_in", ()):
            kind = "ExternalInput"
        elif name in debug.get("ext_out", ()):
            kind = "ExternalOutput"
        K[name] = nc.dram_tensor(name + "_scr", list(shape), dt, kind=kind).ap()

    K["b_const"] = bc = Buf("const")
    K["ones_bf"] = C.sb([128, 128], BF16, "ones")
    K["ones_f"] = C.sb([128, 128], F32, "onesf")
    K["ident_f"] = C.sb([128, 128], F32, "identf")
    K["ident_b"] = C.sb([128, 128], BF16, "identb")
    K["U_f"] = C.sb([128, 128], F32, "U")
    K["SL_f"] = C.sb([128, 128], F32, "SL")
    K["eps_sb"] = C.sb([128, 1], F32, "eps")
    P.op("dve", lambda e: e.memset(K["ones_bf"][:], 1.0), writes=[bc])
    P.op("dve", lambda e: e.memset(K["ones_f"][:], 1.0), writes=[bc])
    P.op("dve", lambda e: e.memset(K["eps_sb"][:], EPS), writes=[bc])
    P.dma("sp", K["ident_f"][:], K["c_ident_f"][:, :], writes=[bc])
    P.dma("sp", K["U_f"][:], K["c_U"][:, :], writes=[bc])
    P.dma("sp", K["SL_f"][:], K["c_SL"][:, :], writes=[bc])
    P.op("dve", lambda e: e.tensor_copy(out=K["ident_b"][:], in_=K["ident_f"][:]), reads=[bc], writes=[bc])
    P.barrier()

    only = debug.get("only")
    nl = debug.get("nl", DEPTH)
    for l in range(nl):
        last = (l == DEPTH - 1)
        if only in (None, "ffn1"):
            ffn_phase(P, C, K, l, 1, src_tm=(x if (l == 0 and "xT" not in debug.get("ext_in", ())) else None),
                      dst_tm=(y if only == "ffn1" else None))
        if only in (None, "inproj"):
            inproj_phase(P, C, K, l)
        if only in (None, "ssd"):
            ssd_phase(P, C, K, l)
        if only in (None, "lru"):
            lru_phase(P, C, K, l)
        if only in (None, "dsa"):
            dsa_phase(P, C, K, l)
        if only in (None, "ffn2"):
            ffn_phase(P, C, K, l, 2, dst_tm=(y if (last or only == "ffn2") else None), outproj=True)
    P.barrier()
    print("instructions", P.nins, "waits", P.nwait, "counts", P.cnt, flush=True)
    return nc


def host_layout(inputs):
    f = {}
    L = DEPTH
    f32 = np.float32

    def kchunk(w):
        Lk, Kd, N = w.shape
        return np.ascontiguousarray(w.reshape(Lk, Kd // 128, 128, N).transpose(0, 2, 1, 3))

    def pchunk(v):
        Lk, Cn = v.shape
        return np.ascontiguousarray(v.reshape(Lk, Cn // 128, 128).transpose(0, 2, 1))

    def bcast(v, n=128):
        return np.ascontiguousarray(np.broadcast_to(v[:, None, :], (v.shape[0], n, v.shape[1])))

    for which in (1, 2):
        f[f"ffn{which}_w13"] = kchunk(inputs[f"ffn{which}_w13"])
        f[f"ffn{which}_w2"] = kchunk(inputs[f"ffn{which}_w2"])
        f[f"norm_ffn{which}"] = pchunk(inputs[f"norm_ffn{which}"])
    f["norm_mix"] = pchunk(inputs["norm_mix"])
    f["w_in"] = kchunk(inputs["w_in"])
    f["w_out"] = kchunk(inputs["w_out"])
    cw = inputs["ssd_conv_w"]
    f["ssd_conv_w"] = np.ascontiguousarray(cw.reshape(L, 4, 8, 128).transpose(0, 3, 2, 1))
    f["ssd_conv_b"] = pchunk(inputs["ssd_conv_b"])
    f["ssd_dt_bias"] = bcast(inputs["ssd_dt_bias"])
    f["ssd_a_log"] = bcast(inputs["ssd_a_log"])
    f["ssd_d"] = bcast(np.repeat(inputs["ssd_d"], 64, axis=1))
    f["ssd_norm"] = pchunk(inputs["ssd_norm"])
    f["cq_norm"] = pchunk(inputs["cq_norm"])
    f["ckv_norm"] = pchunk(inputs["ckv_norm"])
    f["w_uq"] = kchunk(inputs["w_uq"])
    f["w_qidx"] = kchunk(inputs["w_qidx"])
    f["w_ukv"] = np.ascontiguousarray(inputs["w_ukv"])
    f["q_norm"] = bcast(inputs["q_norm"])
    f["k_norm"] = bcast(inputs["k_norm"])
    f["kidx_norm"] = bcast(inputs["kidx_norm"])
    lw = inputs["lru_conv_w"]
    f["lru_conv_w"] = np.ascontiguousarray(lw.reshape(L, 4, 2, 128).transpose(0, 3, 2, 1))
    f["lru_conv_b"] = pchunk(inputs["lru_conv_b"])
    f["lru_ba"] = pchunk(inputs["lru_ba"])
    f["lru_bi"] = pchunk(inputs["lru_bi"])
    f["lru_lambda"] = pchunk(inputs["lru_lambda"])
    for nm in ("lru_wa", "lru_wi"):
        w = inputs[nm]
        bd = np.zeros((L, 128, 2, 128), f32)
        for cc in range(2):
            for b in range(2):
                bd[:, b * 64:(b + 1) * 64, cc, b * 64:(b + 1) * 64] = w[:, 2 * cc + b]
        f[nm] = bd
    f["c_ident_f"] = np.eye(128, dtype=f32)
    kk = np.arange(128)
    f["c_U"] = (kk[:, None] <= kk[None, :]).astype(f32)
    f["c_SL"] = (kk[:, None] > kk[None, :]).astype(f32)
    inv = (np.float32(500000.0) ** (-(np.arange(8, dtype=f32) * np.float32(2.0) / np.float32(16)))).astype(f32)
    ang = (np.arange(SEQ, dtype=f32)[:, None] * inv[None, :]).astype(f32)
    f["c_cos"] = np.ascontiguousarray(np.cos(ang).astype(f32).reshape(32, 128, 8).transpose(1, 0, 2))
    f["c_sin"] = np.ascontiguousarray(np.sin(ang).astype(f32).reshape(32, 128, 8).transpose(1, 0, 2))
    f["c_cmask"] = np.where(kk[None, :] > kk[:, None], f32(-1.0e30), f32(0.0)).astype(f32)
    f["c_rdiv"] = np.ascontiguousarray(np.broadcast_to(np.array([1.0 / 256, 1.0 / 128, 1.0 / 64], f32)[None, :], (128, 3)))
    f["c_pw2"] = np.ascontiguousarray(np.broadcast_to((0.5 ** np.arange(1, NBIS + 1)).astype(f32)[None, :], (128, NBIS)))
    return f


def kernel(**inputs):
    inputs = {k: np.asarray(v) for k, v in inputs.items()}
    shared = host_layout(inputs)
    nc = build_program()
    x = np.ascontiguousarray(inputs["x"], dtype=np.float32)
    in_maps = []
    for b in range(NCORES):
        mm = dict(shared)
        mm["x"] = x[b]
        in_maps.append(mm)
    res = run_bass_kernel_spmd(nc, in_maps, core_ids=list(range(NCORES)))
    return np.stack([r["y"] for r in res.results], axis=0).astype(np.float32)
```

```python
import numpy as np
import concourse.bass as bass
import concourse.mybir as mybir
from concourse.bass_utils import run_bass_kernel_spmd

F32 = mybir.dt.float32
BF16 = mybir.dt.bfloat16
AF = mybir.ActivationFunctionType
ALU = mybir.AluOpType
AX = mybir.AxisListType

D_MODEL = 1024
SEQ = 4096
DEPTH = 2
D_FF = 2816
NCORES = 8
EPS = 1e-6
IN_COLS = 2508


class Buf:
    __slots__ = ("name", "w", "r")

    def __init__(self, name):
        self.name = name
        self.w = None
        self.r = {}


class Prog:
    ENG = ["pe", "act", "dve", "pool", "sp"]
    NDS = 32
    NSW = 8

    def __init__(self, nc):
        self.nc = nc
        self.eng = dict(pe=nc.tensor, act=nc.scalar, dve=nc.vector, pool=nc.gpsimd, sp=nc.sync)
        self.sem = {e: nc.alloc_semaphore("sem_" + e) for e in self.ENG}
        self.cnt = {e: 0 for e in self.ENG}
        self.dsem = [nc.alloc_semaphore(f"dmas{i}") for i in range(self.NDS)]
        self.dval = [0] * self.NDS
        self.dnext = 0
        self.ssem = [nc.alloc_semaphore(f"dmaw{i}") for i in range(self.NSW)]
        self.sval = [0] * self.NSW
        self.snext = 0
        self.seen = {e: {} for e in self.ENG}
        self.nwait = 0
        self.nins = 0

    def _wait(self, e, tok):
        kind, idx, val = tok
        if kind == "e" and idx == e and e == "pe":
            return
        key = (kind, idx)
        if self.seen[e].get(key, 0) >= val:
            return
        sem = self.sem[idx] if kind == "e" else (self.dsem[idx] if kind == "d" else self.ssem[idx])
        self.eng[e].wait_ge(sem, val)
        self.seen[e][key] = val
        self.nwait += 1

    def _deps(self, e, reads, writes):
        for b in reads:
            if b.w is not None:
                self._wait(e, b.w)
        for b in writes:
            if b.w is not None:
                self._wait(e, b.w)
            for k, v in b.r.items():
                self._wait(e, (k[0], k[1], v))

    def _mark(self, tok, reads, writes):
        key = (tok[0], tok[1])
        for b in reads:
            if b.r.get(key, 0) < tok[2]:
                b.r[key] = tok[2]
        for b in writes:
            b.w = tok
            b.r = {}

    def op(self, e, fn, reads=(), writes=(), inc=True):
        self._deps(e, reads, writes)
        ins = fn(self.eng[e])
        self.nins += 1
        if inc:
            ins.then_inc(self.sem[e], 1)
            self.cnt[e] += 1
            tok = ("e", e, self.cnt[e])
        else:
            assert e == "pe"
            tok = ("e", e, self.cnt[e] + 1)
        self._mark(tok, reads, writes)
        return ins

    def dma(self, q, out, in_, reads=(), writes=(), **kw):
        self._deps(q, reads, writes)
        if q == "pool":
            s = self.snext
            self.snext = (self.snext + 1) % self.NSW
            if self.sval[s] > 0:
                self._wait(q, ("s", s, self.sval[s]))
            ins = self.eng[q].dma_start(out=out, in_=in_, **kw)
            ins.then_inc(self.ssem[s], 16)
            self.sval[s] += 16
            tok = ("s", s, self.sval[s])
        else:
            s = self.dnext
            self.dnext = (self.dnext + 1) % self.NDS
            if self.dval[s] > 0:
                self._wait(q, ("d", s, self.dval[s]))
            ins = self.eng[q].dma_start(out=out, in_=in_, **kw)
            ins.then_inc(self.dsem[s], 16)
            self.dval[s] += 16
            tok = ("d", s, self.dval[s])
        self.nins += 1
        self._mark(tok, reads, writes)
        return ins

    def barrier(self):
        for e in self.ENG:
            for e2 in self.ENG:
                if e2 != e and self.cnt[e2] > 0:
                    self._wait(e, ("e", e2, self.cnt[e2]))
            for s in range(self.NDS):
                if self.dval[s] > 0:
                    self._wait(e, ("d", s, self.dval[s]))
            for s in range(self.NSW):
                if self.sval[s] > 0:
                    self._wait(e, ("s", s, self.sval[s]))


class Ctx:
    def __init__(self, nc):
        self.nc = nc
        self.stack = []
        self.n = 0

    def sb(self, shape, dt, name=None):
        self.n += 1
        g = self.nc.sbuf_tensor(f"{name or 't'}_{self.n}", list(shape), dt)
        t = g.__enter__()
        self.stack.append(g)
        return t

    def ps(self, shape, dt=F32, name=None):
        self.n += 1
        g = self.nc.psum_tensor(f"{name or 'p'}_{self.n}", list(shape), dt)
        t = g.__enter__()
        self.stack.append(g)
        return t

    def mark(self):
        return len(self.stack)

    def release(self, m):
        while len(self.stack) > m:
            g = self.stack.pop()
            g.__exit__(None, None, None)


def ffn_phase(P, C, K, l, which, src_tm=None, dst_tm=None, outproj=False):
    nc = P.nc
    TT = 256
    NT = SEQ // TT
    m = C.mark()
    w13_d = K[f"ffn{which}_w13"][l]
    w2_d = K[f"ffn{which}_w2"][l]
    g_d = K[f"norm_ffn{which}"][l]
    xT = K["xT"]

    w13 = C.sb([128, 8, 2 * D_FF], BF16, "w13")
    w2 = C.sb([128, 22, D_MODEL], BF16, "w2")
    g_sb = C.sb([128, 8], F32, "g")
    b_w13 = [Buf(f"w13_{c}") for c in range(8)]
    b_w2 = [Buf(f"w2_{j}") for j in range(22)]
    b_g = Buf("g")
    for c in range(8):
        P.dma("pool", w13[:, c, :], w13_d[:, c, :], writes=[b_w13[c]])
    for j0 in range(0, 22, 2):
        P.dma("pool", w2[:, j0:j0 + 2, :], w2_d[:, j0:j0 + 2, :], writes=[b_w2[j0], b_w2[j0 + 1]])
    P.dma("sp", g_sb[:], g_d[:, :], writes=[b_g])

    if outproj:
        wo = C.sb([128, 8, D_MODEL], BF16, "wo")
        b_wo = Buf("wo")
        P.dma("sp", wo[:], K["w_out_s"][:, :, :], writes=[b_wo])
        mx = [C.sb([128, 8, TT], BF16, "mx") for _ in range(2)]
        b_mx = [Buf("mx0"), Buf("mx1")]
        mixed_v = K["mixedT"].rearrange("(c p) t -> p c t", p=128)
    x_sb = [C.sb([128, 8, TT], F32, "x") for _ in range(2)]
    b_x = [Buf("x0"), Buf("x1")]
    xn = C.sb([128, 8, TT], BF16, "xn")
    b_xn = Buf("xn")
    hT = C.sb([128, 22, TT], BF16, "hT")
    b_h = [Buf(f"h{j}") for j in range(22)]
    sg = [C.sb([128, TT], F32, "sg") for _ in range(2)]
    b_sg = [Buf("sg0"), Buf("sg1")]
    rstd = C.sb([128, TT], F32, "rstd")
    b_rstd = Buf("rstd")
    ps_ss = C.ps([128, 512], F32, "ss")
    b_ss = Buf("ss")
    ps_g = [C.ps([128, 512], F32, "pg") for _ in range(2)]
    ps_u = [C.ps([128, 512], F32, "pu") for _ in range(2)]
    b_pg = [Buf("pg0"), Buf("pg1")]
    b_pu = [Buf("pu0"), Buf("pu1")]
    ps_o = [C.ps([128, 512], F32, "po") for _ in range(2)]
    b_po = [Buf("po0"), Buf("po1")]
    if src_tm is not None:
        xtm = C.sb([128, 2, D_MODEL], F32, "xtm")
        b_xtm = Buf("xtm")
    if dst_tm is not None:
        ytm = C.sb([128, 2, D_MODEL], F32, "ytm")
        b_ytm = Buf("ytm")
    ones_bf = K["ones_bf"]
    ident_f = K["ident_f"]
    eps_sb = K["eps_sb"]
    b_const = K["b_const"]
    oc = 0
    for it in range(NT):
        t0 = it * TT
        xb = x_sb[it % 2]
        bx = b_x[it % 2]
        if src_tm is None:
            P.dma("sp", xb[:], xT[:, :, t0:t0 + TT].rearrange("c p t -> p c t"), writes=[bx])
        else:
            P.dma("sp", xtm[:], src_tm[t0:t0 + TT, :].rearrange("(s p) d -> p s d", p=128), writes=[b_xtm])
            for c2 in range(4):
                pst = ps_o[oc % 2]
                bpo = b_po[oc % 2]
                oc += 1
                for cc in range(2):
                    c = c2 * 2 + cc
                    for s in range(2):
                        P.op("pe", lambda e, c=c, s=s, cc=cc, pst=pst: e.transpose(
                            pst[:, cc * 256 + s * 128: cc * 256 + (s + 1) * 128], xtm[:, s, c * 128:(c + 1) * 128], ident_f[:]),
                            reads=[b_xtm, b_const], writes=[bpo], inc=(cc == 1 and s == 1))
                P.op("act", lambda e, c2=c2, pst=pst: e.copy(out=xb[:, c2 * 2:c2 * 2 + 2, :], in_=pst[:, 0:512].rearrange("p (c t) -> p c t", c=2)),
                     reads=[bpo], writes=[bx])
        if outproj:
            mxb, bmx = mx[it % 2], b_mx[it % 2]
            P.dma("sp", mxb[:], mixed_v[:, :, t0:t0 + TT], writes=[bmx])
            for dc in range(8):
                po = ps_o[oc % 2]
                bpo = b_po[oc % 2]
                oc += 1
                for c in range(8):
                    P.op("pe", lambda e, c=c, dc=dc, po=po, mxb=mxb: e.matmul(po[:, 0:TT], lhsT=wo[:, c, dc * 128:(dc + 1) * 128], rhs=mxb[:, c, :],
                                                                           start=(c == 0), stop=(c == 7)),
                         reads=[bmx, b_wo], writes=[bpo], inc=(c == 7))
                P.op("dve", lambda e, dc=dc, po=po: e.tensor_tensor(out=xb[:, dc, :], in0=xb[:, dc, :], in1=po[:, 0:TT], op=ALU.add),
                     reads=[bpo, bx], writes=[bx])
        rms_xn(P, K, xb, bx, g_sb, b_g, xn, b_xn, rstd, b_rstd, ps_ss, b_ss, TT)
        for j in range(22):
            pg, pu = ps_g[j % 2], ps_u[j % 2]
            bg, bu = b_pg[j % 2], b_pu[j % 2]
            for c in range(8):
                P.op("pe", lambda e, c=c, j=j, pg=pg: e.matmul(pg[:, 0:TT], lhsT=w13[:, c, j * 128:(j + 1) * 128], rhs=xn[:, c, :],
                                                            start=(c == 0), stop=(c == 7)),
                     reads=[b_xn, b_w13[c]], writes=[bg], inc=(c == 7))
            for c in range(8):
                P.op("pe", lambda e, c=c, j=j, pu=pu: e.matmul(pu[:, 0:TT], lhsT=w13[:, c, D_FF + j * 128:D_FF + (j + 1) * 128], rhs=xn[:, c, :],
                                                            start=(c == 0), stop=(c == 7)),
                     reads=[b_xn, b_w13[c]], writes=[bu], inc=(c == 7))
            sgb, bsg = sg[j % 2], b_sg[j % 2]
            P.op("act", lambda e, pg=pg, sgb=sgb: e.activation(out=sgb[:], in_=pg[:, 0:TT], func=AF.Silu), reads=[bg], writes=[bsg])
            P.op("dve", lambda e, j=j, pu=pu, sgb=sgb: e.tensor_tensor(out=hT[:, j, :], in0=sgb[:], in1=pu[:, 0:TT], op=ALU.mult),
                 reads=[bsg, bu], writes=[b_h[j]])
        for dc in range(8):
            po = ps_o[oc % 2]
            bpo = b_po[oc % 2]
            oc += 1
            for j in range(22):
                P.op("pe", lambda e, j=j, dc=dc, po=po: e.matmul(po[:, 0:TT], lhsT=w2[:, j, dc * 128:(dc + 1) * 128], rhs=hT[:, j, :],
                                                              start=(j == 0), stop=(j == 21)),
                     reads=[b_h[j], b_w2[j]], writes=[bpo], inc=(j == 21))
            P.op("dve", lambda e, dc=dc, po=po: e.scalar_tensor_tensor(out=xb[:, dc, :], in0=po[:, 0:TT], scalar=0.5, in1=xb[:, dc, :],
                                                                    op0=ALU.mult, op1=ALU.add),
                 reads=[bpo, bx], writes=[bx])
        if dst_tm is None:
            P.dma("sp", xT[:, :, t0:t0 + TT].rearrange("c p t -> p c t"), xb[:], reads=[bx])
        else:
            for c2 in range(4):
                pst = ps_o[oc % 2]
                bpo = b_po[oc % 2]
                oc += 1
                for s in range(2):
                    for cc in range(2):
                        c = c2 * 2 + cc
                        P.op("pe", lambda e, c=c, s=s, cc=cc, pst=pst: e.transpose(
                            pst[:, s * 256 + cc * 128: s * 256 + (cc + 1) * 128], xb[:, c, s * 128:(s + 1) * 128], ident_f[:]),
                            reads=[bx, b_const], writes=[bpo], inc=(cc == 1 and s == 1))
                P.op("act", lambda e, c2=c2, pst=pst: e.copy(out=ytm[:, :, c2 * 256:(c2 + 1) * 256], in_=pst[:, 0:512].rearrange("p (s d) -> p s d", s=2)),
                     reads=[bpo], writes=[b_ytm])
            P.dma("sp", dst_tm[t0:t0 + TT, :].rearrange("(s p) d -> p s d", p=128), ytm[:], reads=[b_ytm])
    P.barrier()
    C.release(m)


def load_cast_scaled(P, C, dst_bf, b_dst, src_d, shape, scale_sb=None, b_scale=None, nchunk=None):
    if scale_sb is None:
        P.dma("pool", dst_bf, src_d, writes=[b_dst])
        return
    m = C.mark()
    st = C.sb(shape, F32, "wst")
    bs = Buf("wst")
    P.dma("sp", st[:], src_d, writes=[bs])
    if nchunk is None:
        P.op("dve", lambda e: e.tensor_scalar(out=dst_bf, in0=st[:], scalar1=scale_sb[:, 0:1], scalar2=None, op0=ALU.mult),
             reads=[bs, b_scale], writes=[b_dst])
    else:
        for c in range(nchunk):
            P.op("dve", lambda e, c=c: e.tensor_scalar(out=dst_bf[:, c, :], in0=st[:, c, :], scalar1=scale_sb[:, c:c + 1], scalar2=None, op0=ALU.mult),
                 reads=[bs, b_scale], writes=[b_dst])
    P.barrier()
    C.release(m)


def rms_xn(P, K, xb, bx, g_sb, b_g, xn, b_xn, rstd, b_rstd, ps_ss, b_ss, TT):
    ones_bf, eps_sb, b_const = K["ones_bf"], K["eps_sb"], K["b_const"]
    P.op("act", lambda e: e.activation(out=xn[:], in_=xb[:], func=AF.Square), reads=[bx], writes=[b_xn])
    for c in range(8):
        P.op("pe", lambda e, c=c: e.matmul(ps_ss[:, 0:TT], lhsT=ones_bf[:], rhs=xn[:, c, :], start=(c == 0), stop=(c == 7)),
             reads=[b_xn, b_const], writes=[b_ss], inc=(c == 7))
    P.op("act", lambda e: e.activation(out=rstd[:], in_=ps_ss[:, 0:TT], func=AF.Sqrt, bias=eps_sb[:, 0:1], scale=1.0 / D_MODEL),
         reads=[b_ss, b_const], writes=[b_rstd])
    P.op("dve", lambda e: e.reciprocal(out=rstd[:], in_=rstd[:]), reads=[b_rstd], writes=[b_rstd])
    for c in range(8):
        P.op("dve", lambda e, c=c: e.scalar_tensor_tensor(out=xn[:, c, :], in0=xb[:, c, :], scalar=g_sb[:, c:c + 1], in1=rstd[:],
                                                          op0=ALU.mult, op1=ALU.mult),
             reads=[bx, b_g, b_rstd], writes=[b_xn])


def inproj_phase(P, C, K, l):
    TT = 512
    NT = SEQ // TT
    m = C.mark()
    w_in = C.sb([128, 8, IN_COLS], BF16, "w_in")
    b_w = Buf("w_in")
    for c in range(8):
        P.dma("pool", w_in[:, c, :], K["w_in"][l][:, c, :], writes=[b_w])
    g_sb = C.sb([128, 8], F32, "g")
    b_g = Buf("g")
    P.dma("sp", g_sb[:], K["norm_mix"][l][:, :], writes=[b_g])
    x_sb = [C.sb([128, 8, TT], F32, "x") for _ in range(2)]
    b_x = [Buf("x0"), Buf("x1")]
    xn = C.sb([128, 8, TT], BF16, "xn")
    b_xn = Buf("xn")
    rstd = C.sb([128, TT], F32, "rstd")
    b_rstd = Buf("rstd")
    ps_ss = C.ps([128, 512], F32, "ss")
    b_ss = Buf("ss")
    ps_fm = [C.ps([128, 512], F32, "pfm") for _ in range(3)]
    b_pfm = [Buf(f"pfm{i}") for i in range(3)]
    ps_tm = [C.ps([128, 512], F32, "ptm") for _ in range(3)]
    b_ptm = [Buf(f"ptm{i}") for i in range(3)]
    NST = 4
    st_f = [C.sb([128, 512], F32, "stf") for _ in range(NST)]
    b_stf = [Buf(f"stf{i}") for i in range(NST)]
    st_b = [C.sb([128, 512], BF16, "stb") for _ in range(2)]
    b_stb = [Buf(f"stb{i}") for i in range(2)]
    xT = K["xT"]
    xbcT = K["xbcT"]
    cqkvT = K["cqkvT"]
    xlglT = K["xlglT"]
    fm = []
    for i in range(8):
        fm.append((512 + i * 128, xbcT, i * 128, False))
    for i in range(2):
        fm.append((1544 + i * 128, cqkvT, i * 128, True))
    fm.append((1800, cqkvT, 256, True))
    for i in range(4):
        fm.append((1996 + i * 128, xlglT, i * 128, False))
    fi = 0
    ti = 0
    si = 0
    sbi = 0
    for it in range(NT):
        t0 = it * TT
        xb, bx = x_sb[it % 2], b_x[it % 2]
        P.dma("sp", xb[:], xT[:, :, t0:t0 + TT].rearrange("c p t -> p c t"), writes=[bx])
        rms_xn(P, K, xb, bx, g_sb, b_g, xn, b_xn, rstd, b_rstd, ps_ss, b_ss, TT)
        for (col0, dst, row0, isb) in fm:
            ps, bp = ps_fm[fi % 3], b_pfm[fi % 3]
            fi += 1
            for c in range(8):
                P.op("pe", lambda e, c=c, ps=ps, col0=col0: e.matmul(ps[:, 0:TT], lhsT=w_in[:, c, col0:col0 + 128], rhs=xn[:, c, :],
                                                                  start=(c == 0), stop=(c == 7)),
                     reads=[b_xn, b_w], writes=[bp], inc=(c == 7))
            if isb:
                st, bs = st_b[sbi % 2], b_stb[sbi % 2]
                sbi += 1
                P.op("dve", lambda e, st=st, ps=ps: e.tensor_copy(out=st[:, 0:TT], in_=ps[:, 0:TT]), reads=[bp], writes=[bs])
            else:
                st, bs = st_f[si % NST], b_stf[si % NST]
                si += 1
                P.op("act", lambda e, st=st, ps=ps: e.copy(out=st[:, 0:TT], in_=ps[:, 0:TT]), reads=[bp], writes=[bs])
            P.dma("sp", dst[row0:row0 + 128, t0:t0 + TT], st[:, 0:TT], reads=[bs])
        for s in range(4):
            for (col0, ncol, dst) in ((0, 512, K["z_tm"]), (1536, 460, K["att_tm"])):
                ps, bp = ps_tm[ti % 3], b_ptm[ti % 3]
                ti += 1
                for c in range(8):
                    P.op("pe", lambda e, c=c, ps=ps, col0=col0, ncol=ncol, s=s: e.matmul(
                        ps[:, 0:ncol], lhsT=xn[:, c, s * 128:(s + 1) * 128], rhs=w_in[:, c, col0:col0 + ncol], start=(c == 0), stop=(c == 7)),
                        reads=[b_xn, b_w], writes=[bp], inc=(c == 7))
                st, bs = st_f[si % NST], b_stf[si % NST]
                si += 1
                if (ti % 2) == 0:
                    P.op("act", lambda e, st=st, ps=ps, ncol=ncol: e.copy(out=st[:, 0:ncol], in_=ps[:, 0:ncol]), reads=[bp], writes=[bs])
                else:
                    P.op("dve", lambda e, st=st, ps=ps, ncol=ncol: e.tensor_copy(out=st[:, 0:ncol], in_=ps[:, 0:ncol]), reads=[bp], writes=[bs])
                P.dma("sp", dst[t0 + s * 128:t0 + (s + 1) * 128, :], st[:, 0:ncol], reads=[bs])
    P.barrier()
    C.release(m)


def softplus_small(P, C, x, bx, shape, tmp_names="sp"):
    t = C.sb(shape, F32, "spt")
    bt = Buf("spt")
    P.op("act", lambda e: e.activation(out=t[:], in_=x, func=AF.Abs), reads=[bx], writes=[bt])
    P.op("act", lambda e: e.activation(out=t[:], in_=t[:], func=AF.Exp, scale=-1.0), reads=[bt], writes=[bt])
    P.op("act", lambda e: e.activation(out=t[:], in_=t[:], func=AF.Ln, bias=1.0), reads=[bt], writes=[bt])
    P.op("dve", lambda e: e.scalar_tensor_tensor(out=x, in0=x, scalar=0.0, in1=t[:], op0=ALU.max, op1=ALU.add), reads=[bx, bt], writes=[bx])


def ssd_phase(P, C, K, l):
    TT = 512
    NT = SEQ // TT
    m = C.mark()
    bc = K["b_const"]
    U, SL, ones_f, ident_b = K["U_f"], K["SL_f"], K["ones_f"], K["ident_b"]
    eps_sb = K["eps_sb"]
    m2 = C.mark()
    wo_f = C.sb([128, 8, D_MODEL], F32, "wo_f")
    wo_b = C.sb([128, 8, D_MODEL], BF16, "wo_b")
    gss = C.sb([128, 4], F32, "gss")
    b_wo = Buf("wo")
    P.dma("sp", wo_f[:], K["w_out"][l][:, :, :], writes=[b_wo])
    P.dma("sp", gss[:], K["ssd_norm"][l][:, :], writes=[b_wo])
    for c in range(8):
        if c < 4:
            P.op("dve", lambda e, c=c: e.tensor_scalar(out=wo_b[:, c, :], in0=wo_f[:, c, :], scalar1=gss[:, c:c + 1], scalar2=None, op0=ALU.mult),
                 reads=[b_wo], writes=[b_wo])
        else:
            P.op("act", lambda e, c=c: e.copy(out=wo_b[:, c, :], in_=wo_f[:, c, :]), reads=[b_wo], writes=[b_wo])
    P.dma("sp", K["w_out_s"][:, :, :], wo_b[:], reads=[b_wo])
    P.barrier()
    C.release(m2)

    cw = C.sb([128, 8, 4], F32, "cw")
    cb = C.sb([128, 8], F32, "cb")
    dtb = C.sb([128, 8], F32, "dtb")
    aneg = C.sb([128, 8], F32, "aneg")
    Dbc = C.sb([128, 512], F32, "Dbc")
    b_par = Buf("par")
    P.dma("sp", cw[:], K["ssd_conv_w"][l][:, :, :], writes=[b_par])
    P.dma("sp", cb[:], K["ssd_conv_b"][l][:, :], writes=[b_par])
    P.dma("sp", dtb[:], K["ssd_dt_bias"][l][:, :], writes=[b_par])
    P.dma("sp", aneg[:], K["ssd_a_log"][l][:, :], writes=[b_par])
    P.dma("sp", Dbc[:], K["ssd_d"][l][:, :], writes=[b_par])
    P.op("act", lambda e: e.activation(out=aneg[:], in_=aneg[:], func=AF.Exp), reads=[b_par], writes=[b_par])
    P.op("dve", lambda e: e.tensor_scalar(out=aneg[:], in0=aneg[:], scalar1=-1.0, scalar2=None, op0=ALU.mult), reads=[b_par], writes=[b_par])
    dt = C.sb([128, 32, 8], F32, "dt")
    adt = C.sb([128, 32, 8], F32, "adt")
    acum = C.sb([128, 32, 8], F32, "acum")
    tot = C.sb([128, 32, 8], F32, "tot")
    eacum = C.sb([128, 32, 8], F32, "eacum")
    dlast = C.sb([128, 32, 8], F32, "dlast")
    cdec = C.sb([128, 32, 8], F32, "cdec")
    b_dt = Buf("dt")
    b_su = Buf("su")
    for i in range(4):
        P.dma("sp", dt[:, i * 8:(i + 1) * 8, :], K["att_tm"][i * 1024:(i + 1) * 1024, 0:8].rearrange("(n p) h -> p n h", p=128), writes=[b_dt])
    P.op("dve", lambda e: e.tensor_tensor(out=dt[:], in0=dt[:], in1=dtb[:, :].unsqueeze(1).to_broadcast([128, 32, 8]), op=ALU.add),
         reads=[b_dt, b_par], writes=[b_dt])
    softplus_small(P, C, dt[:], b_dt, [128, 32, 8])
    P.op("dve", lambda e: e.tensor_tensor(out=adt[:], in0=dt[:], in1=aneg[:, :].unsqueeze(1).to_broadcast([128, 32, 8]), op=ALU.mult),
         reads=[b_dt, b_par], writes=[b_su])
    ps_seg = [C.ps([128, 512], F32, "pseg") for _ in range(2)]
    b_pseg = [Buf("pseg0"), Buf("pseg1")]
    adt2 = adt[:].rearrange("p n h -> p (n h)")
    P.op("pe", lambda e: e.matmul(ps_seg[0][:, 0:256], lhsT=U[:], rhs=adt2, start=True, stop=True), reads=[b_su, bc], writes=[b_pseg[0]])
    P.op("pe", lambda e: e.matmul(ps_seg[1][:, 0:256], lhsT=ones_f[:], rhs=adt2, start=True, stop=True), reads=[b_su, bc], writes=[b_pseg[1]])
    b_ac = Buf("acum")
    P.op("dve", lambda e: e.tensor_copy(out=acum[:].rearrange("p n h -> p (n h)"), in_=ps_seg[0][:, 0:256]), reads=[b_pseg[0]], writes=[b_ac])
    P.op("dve", lambda e: e.tensor_copy(out=tot[:].rearrange("p n h -> p (n h)"), in_=ps_seg[1][:, 0:256]), reads=[b_pseg[1]], writes=[b_ac])
    P.op("act", lambda e: e.activation(out=eacum[:], in_=acum[:], func=AF.Exp), reads=[b_ac], writes=[b_su])
    P.op("act", lambda e: e.activation(out=cdec[:], in_=tot[:], func=AF.Exp), reads=[b_ac], writes=[b_su])
    P.op("dve", lambda e: e.tensor_tensor(out=dlast[:], in0=tot[:], in1=acum[:], op=ALU.subtract), reads=[b_ac], writes=[b_su])
    P.op("act", lambda e: e.activation(out=dlast[:], in_=dlast[:], func=AF.Exp), reads=[b_su], writes=[b_su])

    xin = [C.sb([128, 8, TT + 3], F32, "xin") for _ in range(2)]
    b_xin = [Buf("xin0"), Buf("xin1")]
    acc = C.sb([128, 8, TT], F32, "acc")
    b_acc = [Buf(f"acc{c}") for c in range(8)]
    xc = C.sb([128, 8, TT], BF16, "xc")
    b_xc = Buf("xc")
    xsB = [C.sb([128, 768], BF16, "xsB") for _ in range(2)]
    b_xsB = [Buf("xsB0"), Buf("xsB1")]
    xsdt = [C.sb([128, 512], BF16, "xsdt") for _ in range(2)]
    b_xsdt = [Buf("xsdt0"), Buf("xsdt1")]
    xsdl = [C.sb([128, 512], BF16, "xsdl") for _ in range(2)]
    b_xsdl = [Buf("xsdl0"), Buf("xsdl1")]
    CBm = C.sb([128, 2, 128], F32, "CBm")
    b_CBm = Buf("CBm")
    NA = 4
    A_h = [C.sb([128, 128], F32, "A") for _ in range(NA)]
    b_A = [Buf(f"A{i}") for i in range(NA)]
    Lt = [C.sb([128, 128], F32, "Lt") for _ in range(NA)]
    b_Lt = [Buf(f"Lt{i}") for i in range(NA)]
    Mt = [C.sb([128, 128], BF16, "Mt") for _ in range(NA)]
    b_Mt = [Buf(f"Mt{i}") for i in range(NA)]
    S_f = C.sb([128, 2, 256], F32, "S_f")
    S_b = C.sb([128, 2, 256], BF16, "S_b")
    b_Sf = Buf("S_f")
    b_Sb = Buf("S_b")
    P.op("dve", lambda e: e.memset(S_f[:], 0.0), writes=[b_Sf])
    P.op("dve", lambda e: e.memset(S_b[:], 0.0), writes=[b_Sb])
    t1 = C.sb([128, 512], F32, "t1")
    b_t1 = Buf("t1")
    xsD = C.sb([128, 512], F32, "xsD")
    b_xsD = Buf("xsD")
    yb = C.sb([128, 512], F32, "y")
    b_y = Buf("y")
    z_sb = [C.sb([128, 512], F32, "z") for _ in range(2)]
    b_z = [Buf("z0"), Buf("z1")]
    ssq = C.sb([128, 2], F32, "ssq")
    b_ssq = Buf("ssq")
    junk = C.sb([128, 256], F32, "junk")
    b_junk = Buf("junk")
    ygn = C.sb([128, 512], BF16, "ygn")
    b_ygn = Buf("ygn")
    ygT = [C.sb([128, 4, TT], BF16, "ygT") for _ in range(2)]
    b_ygT = [Buf("ygT0"), Buf("ygT1")]
    ps_T = C.ps([128, 768], BF16, "psT")
    b_psT = Buf("psT")
    ps_cb = C.ps([128, 512], F32, "pscb")
    b_pscb = Buf("pscb")
    ps_yd = C.ps([128, 512], F32, "psyd")
    b_psyd = Buf("psyd")
    ps_yo = C.ps([128, 512], F32, "psyo")
    b_psyo = Buf("psyo")
    ps_st = C.ps([128, 512], F32, "psst")
    b_psst = Buf("psst")
    ps_yT = C.ps([128, 512], BF16, "psyT")
    b_psyT = Buf("psyT")
    xbc_v = K["xbcT"].rearrange("(c p) t -> p c t", p=128)
    mixed_v = K["mixedT"].rearrange("(c p) t -> p c t", p=128)
    ai = 0
    for it in range(NT):
        t0 = it * TT
        xi, bxi = xin[it % 2], b_xin[it % 2]
        if it == 0:
            P.op("dve", lambda e, xi=xi: e.memset(xi[:, :, 0:3], 0.0), writes=[bxi])
            P.dma("sp", xi[:, :, 3:TT + 3], xbc_v[:, :, 0:TT], writes=[bxi])
        else:
            P.dma("sp", xi[:], xbc_v[:, :, t0 - 3:t0 + TT], writes=[bxi])
        for c in range(8):
            P.op("dve", lambda e, c=c, xi=xi: e.tensor_scalar(out=acc[:, c, :], in0=xi[:, c, 3:TT + 3], scalar1=cw[:, c, 3:4], scalar2=cb[:, c:c + 1],
                                                           op0=ALU.mult, op1=ALU.add), reads=[bxi, b_par], writes=[b_acc[c]])
            for k in range(3):
                P.op("dve", lambda e, c=c, k=k, xi=xi: e.scalar_tensor_tensor(out=acc[:, c, :], in0=xi[:, c, k:TT + k], scalar=cw[:, c, k:k + 1], in1=acc[:, c, :],
                                                                           op0=ALU.mult, op1=ALU.add), reads=[bxi, b_par, b_acc[c]], writes=[b_acc[c]])
        P.op("act", lambda e: e.activation(out=xc[:], in_=acc[:], func=AF.Silu), reads=b_acc, writes=[b_xc])
        for q in range(4):
            n = it * 4 + q
            qs = slice(q * 128, (q + 1) * 128)
            xb_, bxb = xsB[n % 2], b_xsB[n % 2]
            xd, bxd = xsdt[n % 2], b_xsdt[n % 2]
            xl_, bxl = xsdl[n % 2], b_xsdl[n % 2]
            zb, bz = z_sb[n % 2], b_z[n % 2]
            P.dma("sp", zb[:], K["z_tm"][n * 128:(n + 1) * 128, :], writes=[bz])
            for cc in range(6):
                P.op("pe", lambda e, cc=cc, qs=qs: e.transpose(ps_T[:, cc * 128:(cc + 1) * 128], xc[:, cc, qs], ident_b[:]),
                     reads=[b_xc, bc], writes=[b_psT], inc=(cc == 5))
            P.op("act", lambda e, xb_=xb_: e.copy(out=xb_[:], in_=ps_T[:]), reads=[b_psT], writes=[bxb])
            P.op("dve", lambda e, xb_=xb_, xd=xd, n=n: e.tensor_tensor(out=xd[:].rearrange("p (h d) -> p h d", h=8),
                                                                       in0=xb_[:, 0:512].rearrange("p (h d) -> p h d", h=8),
                                                                       in1=dt[:, n, :].unsqueeze(2).to_broadcast([128, 8, 64]), op=ALU.mult),
                 reads=[bxb, b_dt], writes=[bxd])
            P.op("pool", lambda e, xl_=xl_, xd=xd, n=n: e.tensor_tensor(out=xl_[:].rearrange("p (h d) -> p h d", h=8),
                                                                        in0=xd[:].rearrange("p (h d) -> p h d", h=8),
                                                                        in1=dlast[:, n, :].unsqueeze(2).to_broadcast([128, 8, 64]), op=ALU.mult),
                 reads=[bxd, b_su], writes=[bxl])
            for g in range(2):
                P.op("pe", lambda e, g=g, qs=qs: e.matmul(ps_cb[:, g * 128:(g + 1) * 128], lhsT=xc[:, 4 + g, qs], rhs=xc[:, 6 + g, qs], start=True, stop=True),
                     reads=[b_xc], writes=[b_pscb], inc=(g == 1))
            P.op("dve", lambda e: e.tensor_tensor(out=CBm[:], in0=ps_cb[:, 0:256].rearrange("p (g i) -> p g i", g=2),
                                                  in1=U[:, :].unsqueeze(1).to_broadcast([128, 2, 128]), op=ALU.mult),
                 reads=[b_pscb, bc], writes=[b_CBm])
            for h in range(8):
                a_, ba_ = A_h[ai % NA], b_A[ai % NA]
                l_, bl_ = Lt[ai % NA], b_Lt[ai % NA]
                m_, bm_ = Mt[ai % NA], b_Mt[ai % NA]
                ai += 1
                pseg, bps = ps_seg[h // 4], b_pseg[h // 4]
                hs = slice((h % 4) * 128, (h % 4 + 1) * 128)
                P.op("pool", lambda e, a_=a_, n=n, h=h: e.tensor_scalar(out=a_[:], in0=SL[:], scalar1=adt[:, n, h:h + 1], scalar2=0.0, op0=ALU.mult, op1=ALU.add),
                     reads=[bc, b_su], writes=[ba_])
                P.op("pe", lambda e, a_=a_, pseg=pseg, hs=hs: e.matmul(pseg[:, hs], lhsT=a_[:], rhs=U[:], start=True, stop=True),
                     reads=[ba_, bc], writes=[bps])
                P.op("act", lambda e, l_=l_, pseg=pseg, hs=hs: e.activation(out=l_[:], in_=pseg[:, hs], func=AF.Exp), reads=[bps], writes=[bl_])
                P.op("dve", lambda e, m_=m_, l_=l_, h=h: e.tensor_tensor(out=m_[:], in0=CBm[:, h // 4, :], in1=l_[:], op=ALU.mult),
                     reads=[b_CBm, bl_], writes=[bm_])
                P.op("pe", lambda e, m_=m_, xd=xd, h=h: e.matmul(ps_yd[:, h * 64:(h + 1) * 64], lhsT=m_[:], rhs=xd[:, h * 64:(h + 1) * 64], start=True, stop=True),
                     reads=[bm_, bxd], writes=[b_psyd], inc=(h == 7))
            for g in range(2):
                P.op("pe", lambda e, g=g, qs=qs: e.matmul(ps_yo[:, g * 256:(g + 1) * 256], lhsT=xc[:, 6 + g, qs], rhs=S_b[:, g, :], start=True, stop=True),
                     reads=[b_xc, b_Sb], writes=[b_psyo], inc=(g == 1))
            P.op("dve", lambda e, n=n: e.tensor_tensor(out=t1[:].rearrange("p (h d) -> p h d", h=8), in0=ps_yo[:].rearrange("p (h d) -> p h d", h=8),
                                                       in1=eacum[:, n, :].unsqueeze(2).to_broadcast([128, 8, 64]), op=ALU.mult),
                 reads=[b_psyo, b_su], writes=[b_t1])
            P.op("pool", lambda e, xb_=xb_: e.tensor_tensor(out=xsD[:], in0=xb_[:, 0:512], in1=Dbc[:], op=ALU.mult), reads=[bxb, b_par], writes=[b_xsD])
            P.op("pool", lambda e: e.tensor_tensor(out=t1[:], in0=t1[:], in1=xsD[:], op=ALU.add), reads=[b_t1, b_xsD], writes=[b_t1])
            P.op("dve", lambda e: e.tensor_tensor(out=yb[:], in0=ps_yd[:], in1=t1[:], op=ALU.add), reads=[b_psyd, b_t1], writes=[b_y])
            for g in range(2):
                P.op("pe", lambda e, g=g, xb_=xb_, xl_=xl_: e.matmul(ps_st[:, g * 256:(g + 1) * 256], lhsT=xb_[:, 512 + g * 128:512 + (g + 1) * 128],
                                                                     rhs=xl_[:, g * 256:(g + 1) * 256], start=True, stop=True),
                     reads=[bxb, bxl], writes=[b_psst], inc=(g == 1))
            P.op("dve", lambda e, n=n: e.tensor_tensor(out=S_f[:].rearrange("p g (h d) -> p (g h) d", h=4), in0=S_f[:].rearrange("p g (h d) -> p (g h) d", h=4),
                                                       in1=cdec[:, n, :].unsqueeze(2).to_broadcast([128, 8, 64]), op=ALU.mult),
                 reads=[b_Sf, b_su], writes=[b_Sf])
            P.op("dve", lambda e: e.tensor_tensor(out=S_f[:].rearrange("p g d -> p (g d)"), in0=S_f[:].rearrange("p g d -> p (g d)"), in1=ps_st[:], op=ALU.add),
                 reads=[b_Sf, b_psst], writes=[b_Sf])
            P.op("act", lambda e: e.copy(out=S_b[:], in_=S_f[:]), reads=[b_Sf], writes=[b_Sb])
            P.op("act", lambda e, zb=zb: e.activation(out=zb[:], in_=zb[:], func=AF.Silu), reads=[bz], writes=[bz])
            P.op("dve", lambda e, zb=zb: e.tensor_tensor(out=yb[:], in0=yb[:], in1=zb[:], op=ALU.mult), reads=[b_y, bz], writes=[b_y])
            for g in range(2):
                P.op("act", lambda e, g=g: e.activation(out=junk[:], in_=yb[:, g * 256:(g + 1) * 256], func=AF.Square, accum_out=ssq[:, g:g + 1]),
                     reads=[b_y], writes=[b_junk, b_ssq])
            P.op("act", lambda e: e.activation(out=ssq[:], in_=ssq[:], func=AF.Sqrt, bias=eps_sb[:, 0:1], scale=1.0 / 256.0), reads=[b_ssq, bc], writes=[b_ssq])
            P.op("dve", lambda e: e.reciprocal(out=ssq[:], in_=ssq[:]), reads=[b_ssq], writes=[b_ssq])
            for g in range(2):
                P.op("act", lambda e, g=g: e.mul(out=ygn[:, g * 256:(g + 1) * 256], in_=yb[:, g * 256:(g + 1) * 256], mul=ssq[:, g:g + 1]),
                     reads=[b_y, b_ssq], writes=[b_ygn])
            for cc in range(4):
                P.op("pe", lambda e, cc=cc: e.transpose(ps_yT[:, cc * 128:(cc + 1) * 128], ygn[:, cc * 128:(cc + 1) * 128], ident_b[:]),
                     reads=[b_ygn, bc], writes=[b_psyT], inc=(cc == 3))
            yt, byt = ygT[it % 2], b_ygT[it % 2]
            P.op("dve", lambda e, yt=yt, qs=qs: e.tensor_copy(out=yt[:, :, qs], in_=ps_yT[:].rearrange("p (c t) -> p c t", c=4)), reads=[b_psyT], writes=[byt])
        P.dma("sp", mixed_v[:, 0:4, t0:t0 + TT], ygT[it % 2][:], reads=[b_ygT[it % 2]])
    P.barrier()
    C.release(m)


def lru_phase(P, C, K, l):
    TL = 1024
    NT = SEQ // TL
    m = C.mark()
    bc = K["b_const"]
    lcw = C.sb([128, 2, 4], F32, "lcw")
    lcb = C.sb([128, 2], F32, "lcb")
    ba = C.sb([128, 2], F32, "ba")
    bi = C.sb([128, 2], F32, "bi")
    cL = C.sb([128, 2], F32, "cL")
    Wa = C.sb([128, 2, 128], F32, "Wa")
    Wi = C.sb([128, 2, 128], F32, "Wi")
    b_par = Buf("lpar")
    b_cL = Buf("cL")
    P.dma("sp", lcw[:], K["lru_conv_w"][l][:, :, :], writes=[b_par])
    P.dma("sp", lcb[:], K["lru_conv_b"][l][:, :], writes=[b_par])
    P.dma("sp", ba[:], K["lru_ba"][l][:, :], writes=[b_par])
    P.dma("sp", bi[:], K["lru_bi"][l][:, :], writes=[b_par])
    P.dma("sp", cL[:], K["lru_lambda"][l][:, :], writes=[b_cL])
    P.dma("sp", Wa[:], K["lru_wa"][l][:, :, :], writes=[b_par])
    P.dma("sp", Wi[:], K["lru_wi"][l][:, :, :], writes=[b_par])
    P.op("dve", lambda e: e.tensor_scalar(out=cL[:], in0=cL[:], scalar1=-1.0, scalar2=None, op0=ALU.mult), reads=[b_cL], writes=[b_cL])
    softplus_small(P, C, cL[:], b_cL, [128, 2])
    P.op("dve", lambda e: e.tensor_scalar(out=cL[:], in0=cL[:], scalar1=-8.0, scalar2=None, op0=ALU.mult), reads=[b_cL], writes=[b_cL])
    xin = [C.sb([128, TL + 3], F32, "lxin") for _ in range(2)]
    b_xin = [Buf("lxin0"), Buf("lxin1")]
    gin = [C.sb([128, TL], F32, "gin") for _ in range(2)]
    b_gin = [Buf("gin0"), Buf("gin1")]
    xr = C.sb([128, TL], F32, "xr")
    b_xr = Buf("xr")
    r_ = C.sb([128, TL], F32, "r")
    b_r = Buf("r")
    i_ = C.sb([128, TL], F32, "i")
    b_i = Buf("i")
    a_ = C.sb([128, TL], F32, "a")
    b_a = Buf("a")
    om = C.sb([128, TL], F32, "om")
    b_om = Buf("om")
    hbuf = [[C.sb([128, TL], F32, "h") for _ in range(2)] for _ in range(2)]
    b_h = [[Buf("h00"), Buf("h01")], [Buf("h10"), Buf("h11")]]
    g2 = C.sb([128, TL], F32, "g2")
    b_g2 = Buf("g2")
    yo = [C.sb([128, TL], BF16, "ylru") for _ in range(2)]
    b_yo = [Buf("yo0"), Buf("yo1")]
    ps_r = [C.ps([128, 512], F32, "psr") for _ in range(2)]
    ps_i = [C.ps([128, 512], F32, "psi") for _ in range(2)]
    b_psr = [Buf("psr0"), Buf("psr1")]
    b_psi = [Buf("psi0"), Buf("psi1")]
    xlgl = K["xlglT"]
    mixed = K["mixedT"]
    k = 0
    for it in range(NT):
        t0 = it * TL
        for cc in range(2):
            xi, bxi = xin[k % 2], b_xin[k % 2]
            gi, bgi = gin[k % 2], b_gin[k % 2]
            yo_, byo = yo[k % 2], b_yo[k % 2]
            k += 1
            if it == 0:
                P.op("dve", lambda e, xi=xi: e.memset(xi[:, 0:3], 0.0), writes=[bxi])
                P.dma("sp", xi[:, 3:TL + 3], xlgl[cc * 128:(cc + 1) * 128, 0:TL], writes=[bxi])
            else:
                P.dma("sp", xi[:], xlgl[cc * 128:(cc + 1) * 128, t0 - 3:t0 + TL], writes=[bxi])
            P.dma("sp", gi[:], xlgl[256 + cc * 128:256 + (cc + 1) * 128, t0:t0 + TL], writes=[bgi])
            P.op("dve", lambda e, xi=xi, cc=cc: e.tensor_scalar(out=xr[:], in0=xi[:, 3:TL + 3], scalar1=lcw[:, cc, 3:4], scalar2=lcb[:, cc:cc + 1],
                                                             op0=ALU.mult, op1=ALU.add), reads=[bxi, b_par], writes=[b_xr])
            for kk in range(3):
                P.op("dve", lambda e, xi=xi, cc=cc, kk=kk: e.scalar_tensor_tensor(out=xr[:], in0=xi[:, kk:TL + kk], scalar=lcw[:, cc, kk:kk + 1], in1=xr[:],
                                                                               op0=ALU.mult, op1=ALU.add), reads=[bxi, b_par, b_xr], writes=[b_xr])
            for hf in range(2):
                hs = slice(hf * 512, (hf + 1) * 512)
                P.op("pe", lambda e, hf=hf, hs=hs, cc=cc: e.matmul(ps_r[hf][:], lhsT=Wa[:, cc, :], rhs=xr[:, hs], start=True, stop=True),
                     reads=[b_xr, b_par], writes=[b_psr[hf]])
                P.op("pe", lambda e, hf=hf, hs=hs, cc=cc: e.matmul(ps_i[hf][:], lhsT=Wi[:, cc, :], rhs=xr[:, hs], start=True, stop=True),
                     reads=[b_xr, b_par], writes=[b_psi[hf]])
                P.op("act", lambda e, hf=hf, hs=hs, cc=cc: e.activation(out=r_[:, hs], in_=ps_r[hf][:], func=AF.Sigmoid, bias=ba[:, cc:cc + 1]),
                     reads=[b_psr[hf], b_par], writes=[b_r])
                P.op("act", lambda e, hf=hf, hs=hs, cc=cc: e.activation(out=i_[:, hs], in_=ps_i[hf][:], func=AF.Sigmoid, bias=bi[:, cc:cc + 1]),
                     reads=[b_psi[hf], b_par], writes=[b_i])
            P.op("act", lambda e, cc=cc: e.activation(out=a_[:], in_=r_[:], func=AF.Exp, scale=cL[:, cc:cc + 1]), reads=[b_r, b_cL], writes=[b_a])
            P.op("dve", lambda e: e.tensor_tensor(out=om[:], in0=a_[:], in1=a_[:], op=ALU.mult), reads=[b_a], writes=[b_om])
            P.op("dve", lambda e: e.tensor_scalar(out=om[:], in0=om[:], scalar1=-1.0, scalar2=1.0, op0=ALU.mult, op1=ALU.add), reads=[b_om], writes=[b_om])
            P.op("act", lambda e: e.activation(out=om[:], in_=om[:], func=AF.Sqrt), reads=[b_om], writes=[b_om])
            P.op("pool", lambda e: e.tensor_tensor(out=i_[:], in0=i_[:], in1=xr[:], op=ALU.mult), reads=[b_i, b_xr], writes=[b_i])
            P.op("dve", lambda e: e.tensor_tensor(out=om[:], in0=om[:], in1=i_[:], op=ALU.mult), reads=[b_om, b_i], writes=[b_om])
            hcur, bh = hbuf[cc][it % 2], b_h[cc][it % 2]
            hprev, bhp = hbuf[cc][(it + 1) % 2], b_h[cc][(it + 1) % 2]
            if it == 0:
                P.op("dve", lambda e, hcur=hcur: e.tensor_tensor_scan(out=hcur[:], data0=a_[:], data1=om[:], initial=0.0, op0=ALU.mult, op1=ALU.add),
                     reads=[b_a, b_om], writes=[bh])
            else:
                P.op("dve", lambda e, hcur=hcur, hprev=hprev: e.tensor_tensor_scan(out=hcur[:], data0=a_[:], data1=om[:], initial=hprev[:, TL - 1:TL],
                                                                                 op0=ALU.mult, op1=ALU.add),
                     reads=[b_a, b_om, bhp], writes=[bh])
            P.op("pool", lambda e, gi=gi: e.tensor_tensor(out=g2[:], in0=gi[:], in1=gi[:], op=ALU.mult), reads=[bgi], writes=[b_g2])
            P.op("pool", lambda e: e.tensor_scalar(out=g2[:], in0=g2[:], scalar1=0.044715, scalar2=1.0, op0=ALU.mult, op1=ALU.add), reads=[b_g2], writes=[b_g2])
            P.op("pool", lambda e, gi=gi: e.tensor_tensor(out=g2[:], in0=g2[:], in1=gi[:], op=ALU.mult), reads=[b_g2, bgi], writes=[b_g2])
            P.op("act", lambda e: e.activation(out=g2[:], in_=g2[:], func=AF.Sigmoid, scale=1.5957691216057308), reads=[b_g2], writes=[b_g2])
            P.op("pool", lambda e, gi=gi: e.tensor_tensor(out=g2[:], in0=g2[:], in1=gi[:], op=ALU.mult), reads=[b_g2, bgi], writes=[b_g2])
            P.op("dve", lambda e, hcur=hcur, yo_=yo_: e.tensor_tensor(out=yo_[:], in0=hcur[:], in1=g2[:], op=ALU.mult), reads=[bh, b_g2], writes=[byo])
            P.dma("sp", mixed[768 + cc * 128:768 + (cc + 1) * 128, t0:t0 + TL], yo_[:], reads=[byo])
    P.barrier()
    C.release(m)

NBIS = 16
BIG = 1.0e30


def rope_tm(P, eng, x3, bx, cosn, sinn, nh, tmp, bt, btab):
    cb = cosn.unsqueeze(1).to_broadcast([128, nh, 8])
    sb_ = sinn.unsqueeze(1).to_broadcast([128, nh, 8])
    x1 = x3[:, :, 0:8]
    x2 = x3[:, :, 8:16]
    t1, t2, t3, t4 = tmp[:, 0, 0:nh, :], tmp[:, 1, 0:nh, :], tmp[:, 2, 0:nh, :], tmp[:, 3, 0:nh, :]
    P.op(eng, lambda e: e.tensor_tensor(out=t1, in0=x1, in1=cb, op=ALU.mult), reads=[bx, btab], writes=[bt])
    P.op(eng, lambda e: e.tensor_tensor(out=t2, in0=x2, in1=sb_, op=ALU.mult), reads=[bx, btab], writes=[bt])
    P.op(eng, lambda e: e.tensor_tensor(out=t3, in0=x1, in1=sb_, op=ALU.mult), reads=[bx, btab], writes=[bt])
    P.op(eng, lambda e: e.tensor_tensor(out=t4, in0=x2, in1=cb, op=ALU.mult), reads=[bx, btab], writes=[bt])
    P.op(eng, lambda e: e.tensor_tensor(out=x1, in0=t1, in1=t2, op=ALU.subtract), reads=[bt], writes=[bx])
    P.op(eng, lambda e: e.tensor_tensor(out=x2, in0=t3, in1=t4, op=ALU.add), reads=[bt], writes=[bx])


def dsa_phase(P, C, K, l):
    m = C.mark()
    bc = K["b_const"]
    ident_b, ident_f, eps_sb = K["ident_b"], K["ident_f"], K["eps_sb"]
    NB = SEQ // 128
    gq = C.sb([128, 2], F32, "gq")
    gkv = C.sb([128, 1], F32, "gkv")
    b_g = Buf("gqkv")
    P.dma("sp", gq[:], K["cq_norm"][l][:, :], writes=[b_g])
    P.dma("sp", gkv[:], K["ckv_norm"][l][:, :], writes=[b_g])
    Wq = C.sb([128, 2, 256], BF16, "Wq")
    Wqi = C.sb([128, 2, 256], BF16, "Wqi")
    Wkv = C.sb([128, 512], BF16, "Wkv")
    b_W = Buf("W")
    load_cast_scaled(P, C, Wq, b_W, K["w_uq"][l][:, :, :], [128, 2, 256], gq, b_g, nchunk=2)
    load_cast_scaled(P, C, Wqi, b_W, K["w_qidx"][l][:, :, :], [128, 2, 256], gq, b_g, nchunk=2)
    load_cast_scaled(P, C, Wkv[:], b_W, K["w_ukv"][l][:, :], [128, 512], gkv, b_g)
    qg = C.sb([128, 64], F32, "qg")
    kg = C.sb([128, 64], F32, "kg")
    kig = C.sb([128, 64], F32, "kig")
    cos = C.sb([128, NB, 8], F32, "cos")
    sin = C.sb([128, NB, 8], F32, "sin")
    CM = C.sb([128, 128], F32, "CM")
    rdiv = C.sb([128, 3], F32, "rdiv")
    pw2 = C.sb([128, NBIS], F32, "pw2")
    b_tab = Buf("tab")
    P.dma("sp", qg[:], K["q_norm"][l][:, :], writes=[b_tab])
    P.dma("sp", kg[:], K["k_norm"][l][:, :], writes=[b_tab])
    P.dma("sp", kig[:], K["kidx_norm"][l][:, :], writes=[b_tab])
    P.dma("sp", cos[:], K["c_cos"][:, :, :], writes=[b_tab])
    P.dma("sp", sin[:], K["c_sin"][:, :, :], writes=[b_tab])
    P.dma("sp", CM[:], K["c_cmask"][:, :], writes=[b_tab])
    P.dma("sp", rdiv[:], K["c_rdiv"][:, :], writes=[b_tab])
    P.dma("sp", pw2[:], K["c_pw2"][:, :], writes=[b_tab])
    cqT = C.sb([128, 3, SEQ], BF16, "cqT")
    b_cqT = Buf("cqT")
    cq_v = K["cqkvT"].rearrange("(c p) t -> p c t", p=128)
    for i in range(4):
        P.dma("sp", cqT[:, :, i * 1024:(i + 1) * 1024], cq_v[:, :, i * 1024:(i + 1) * 1024], writes=[b_cqT])
    kT = C.sb([128, 2, SEQ], BF16, "kT")
    b_kT = Buf("kT")
    kiT = C.sb([128, SEQ], F32, "kiT")
    b_kiT = Buf("kiT")
    vaug = C.sb([128, NB, 4, 65], BF16, "vaug")
    b_v = Buf("vaug")
    P.op("pool", lambda e: e.memset(vaug[:], 1.0), writes=[b_v])
    score = C.sb([128, SEQ], F32, "score")
    b_sc = Buf("score")
    mask = C.sb([128, SEQ], BF16, "mask")
    b_mask = Buf("mask")
    junk = C.sb([128, SEQ], BF16, "junkb")
    b_junk = Buf("junkb")
    att = [C.sb([128, 460], F32, "att") for _ in range(2)]
    b_att = [Buf("att0"), Buf("att1")]
    sq = C.sb([128, 256], F32, "sqj")
    b_sq = Buf("sqj")
    st3 = C.sb([128, 3], F32, "st3")
    b_st3 = Buf("st3")
    qs_ = C.sb([128, 4, 64], F32, "qs")
    b_qs = Buf("qs")
    ks_ = C.sb([128, 4, 64], F32, "ks")
    b_ks = Buf("ks")
    qis = C.sb([128, 4, 64], F32, "qis")
    b_qis = Buf("qis")
    ki2 = C.sb([128, 2, 64], F32, "ki2")
    b_ki2 = Buf("ki2")
    sq4 = C.sb([128, 4, 64], F32, "sq4")
    b_sq4 = Buf("sq4")
    r4 = C.sb([128, 8], F32, "r4")
    b_r4 = Buf("r4")
    rtmp = C.sb([128, 4, 4, 8], F32, "rtmp")
    b_rtmp = Buf("rtmp")
    rtmp2 = C.sb([128, 4, 4, 8], F32, "rtmp2")
    b_rtmp2 = Buf("rtmp2")
    wsc = C.sb([128, 4], F32, "wsc")
    lo4 = C.sb([128, 4], F32, "lo4")
    hi4 = C.sb([128, 4], F32, "hi4")
    b_w4 = Buf("w4")
    q_b = C.sb([128, 256], BF16, "q_b")
    b_qb = Buf("q_b")
    k_b = C.sb([128, 256], BF16, "k_b")
    b_kb = Buf("k_b")
    qT = [C.sb([128, 4, 128], BF16, "qTz") for _ in range(2)]
    b_qT = [Buf("qT0"), Buf("qT1")]
    for i in range(2):
        P.op("pool", lambda e, i=i: e.memset(qT[i][:], 0.0), writes=[b_qT[i]])
    qiT = [C.sb([128, 2, 128], F32, "qiT") for _ in range(2)]
    b_qiT = [Buf("qiT0"), Buf("qiT1")]
    clp = [C.sb([128, 512], F32, "clp") for _ in range(2)]
    b_clp = [Buf("clp0"), Buf("clp1")]
    lo = C.sb([128, 1], F32, "lo")
    Wd = C.sb([128, 1], F32, "Wd")
    Wk = C.sb([128, NBIS], F32, "Wk")
    cnt = C.sb([128, 1], F32, "cnt")
    dlt = C.sb([128, 1], F32, "dlt")
    mid = C.sb([128, 1], F32, "mid")
    b_bis = Buf("bis")
    b_cnt = Buf("cnt")
    b_mid = Buf("mid")
    b_dlt = Buf("dlt")
    pT = [C.sb([128, 4, 128], BF16, "pT") for _ in range(3)]
    b_pT = [Buf(f"pT{i}") for i in range(3)]
    rden = C.sb([128, 4], F32, "rden")
    b_rden = Buf("rden")
    yat = C.sb([128, 256], BF16, "yat")
    b_yat = Buf("yat")
    yaT = [C.sb([128, 2, 128], BF16, "yaT") for _ in range(2)]
    b_yaT = [Buf("yaT0"), Buf("yaT1")]
    ps_pr = [C.ps([128, 512], F32, "pspr") for _ in range(2)]
    b_pspr = [Buf("pspr0"), Buf("pspr1")]
    ps_sc = [C.ps([128, 512], F32, "pssc") for _ in range(2)]
    b_pssc = [Buf("pssc0"), Buf("pssc1")]
    ps_tr = C.ps([128, 512], BF16, "pstr")
    b_pstr = Buf("pstr")
    ps_trf = C.ps([128, 512], F32, "pstrf")
    b_pstrf = Buf("pstrf")
    ps_S = [C.ps([128, 512], F32, "psS") for _ in range(1)]
    b_psS = [Buf("psS0")]
    ps_o = C.ps([128, 512], F32, "pso")
    b_pso = Buf("pso")
    ps_mT = ps_tr
    mixed_v = K["mixedT"].rearrange("(c p) t -> p c t", p=128)
    sci = 0
    pti = 0
    dbg = K.get('dbg', {})
    stage = dbg.get('dsa_stage', 9)
    for n in range(dbg.get('dsa_nb', NB)):
        ns = slice(n * 128, (n + 1) * 128)
        at, bat = att[n % 2], b_att[n % 2]
        P.dma("sp", at[:], K["att_tm"][n * 128:(n + 1) * 128, :], writes=[bat])
        cosn, sinn = cos[:, n, :], sin[:, n, :]
        for j, (a0, a1) in enumerate(((8, 264), (264, 392), (392, 456))):
            P.op("act", lambda e, a0=a0, a1=a1, j=j: e.activation(out=sq[:, 0:a1 - a0], in_=at[:, a0:a1], func=AF.Square, accum_out=st3[:, j:j + 1]),
                 reads=[bat], writes=[b_sq, b_st3])
        P.op("dve", lambda e: e.tensor_tensor(out=st3[:], in0=st3[:], in1=rdiv[:], op=ALU.mult), reads=[b_st3, b_tab], writes=[b_st3])
        P.op("act", lambda e: e.activation(out=st3[:], in_=st3[:], func=AF.Sqrt, bias=eps_sb[:, 0:1]), reads=[b_st3, bc], writes=[b_st3])
        P.op("dve", lambda e: e.reciprocal(out=st3[:], in_=st3[:]), reads=[b_st3], writes=[b_st3])
        pq, bpq = ps_pr[0], b_pspr[0]
        pk, bpk = ps_pr[1], b_pspr[1]
        for kc in range(2):
            P.op("pe", lambda e, kc=kc: e.matmul(pq[:, 0:256], lhsT=cqT[:, kc, ns], rhs=Wq[:, kc, :], start=(kc == 0), stop=(kc == 1)),
                 reads=[b_cqT, b_W], writes=[bpq], inc=False)
        for kc in range(2):
            P.op("pe", lambda e, kc=kc: e.matmul(pq[:, 256:512], lhsT=cqT[:, kc, ns], rhs=Wqi[:, kc, :], start=False, stop=(kc == 1), skip_group_check=True),
                 reads=[b_cqT, b_W], writes=[bpq], inc=(kc == 1))
        P.op("pe", lambda e: e.matmul(pk[:, 0:512], lhsT=cqT[:, 2, ns], rhs=Wkv[:], start=True, stop=True), reads=[b_cqT, b_W], writes=[bpk])
        P.op("dve", lambda e: e.tensor_scalar(out=qs_[:].rearrange("p h d -> p (h d)"), in0=pq[:, 0:256], scalar1=st3[:, 0:1], scalar2=None, op0=ALU.mult),
             reads=[bpq, b_st3], writes=[b_qs])
        P.op("dve", lambda e: e.tensor_scalar(out=qis[:].rearrange("p h d -> p (h d)"), in0=pq[:, 256:512], scalar1=st3[:, 0:1], scalar2=None, op0=ALU.mult),
             reads=[bpq, b_st3], writes=[b_qis])
        P.op("dve", lambda e: e.tensor_scalar(out=ks_[:], in0=pk[:, 0:512].rearrange("p (h d) -> p h d", h=4)[:, :, 0:64], scalar1=st3[:, 1:2], scalar2=None, op0=ALU.mult),
             reads=[bpk, b_st3], writes=[b_ks])
        P.op("act", lambda e, n=n: e.mul(out=vaug[:, n, :, 0:64], in_=pk[:, 0:512].rearrange("p (h d) -> p h d", h=4)[:, :, 64:128], mul=st3[:, 1:2]),
             reads=[bpk, b_st3], writes=[b_v])
        for (x_, bx_, c0, gain) in ((qs_, b_qs, 0, qg), (ks_, b_ks, 4, kg)):
            P.op("pool", lambda e, x_=x_: e.tensor_tensor(out=sq4[:], in0=x_[:], in1=x_[:], op=ALU.mult), reads=[bx_], writes=[b_sq4])
            P.op("dve", lambda e, c0=c0: e.tensor_reduce(out=r4[:, c0:c0 + 4], in_=sq4[:], axis=AX.X, op=ALU.add), reads=[b_sq4], writes=[b_r4])
            P.op("act", lambda e, c0=c0: e.activation(out=r4[:, c0:c0 + 4], in_=r4[:, c0:c0 + 4], func=AF.Sqrt, bias=eps_sb[:, 0:1], scale=1.0 / 64.0),
                 reads=[b_r4, bc], writes=[b_r4])
            P.op("dve", lambda e, c0=c0: e.reciprocal(out=r4[:, c0:c0 + 4], in_=r4[:, c0:c0 + 4]), reads=[b_r4], writes=[b_r4])
            P.op("dve", lambda e, x_=x_, c0=c0: e.tensor_tensor(out=x_[:], in0=x_[:], in1=r4[:, c0:c0 + 4].unsqueeze(2).to_broadcast([128, 4, 64]), op=ALU.mult),
                 reads=[bx_, b_r4], writes=[bx_])
            P.op("pool", lambda e, x_=x_, gain=gain: e.tensor_tensor(out=x_[:], in0=x_[:], in1=gain[:, :].unsqueeze(1).to_broadcast([128, 4, 64]), op=ALU.mult),
                 reads=[bx_, b_tab], writes=[bx_])
        rope_tm(P, "dve", qs_[:], b_qs, cosn, sinn, 4, rtmp, b_rtmp, b_tab)
        rope_tm(P, "pool", ks_[:], b_ks, cosn, sinn, 4, rtmp2, b_rtmp2, b_tab)
        P.op("act", lambda e: e.copy(out=q_b[:], in_=qs_[:].rearrange("p h d -> p (h d)")), reads=[b_qs], writes=[b_qb])
        P.op("act", lambda e: e.copy(out=k_b[:], in_=ks_[:].rearrange("p h d -> p (h d)")), reads=[b_ks], writes=[b_kb])
        qT_, bqT_ = qT[n % 2], b_qT[n % 2]
        for pr in range(2):
            P.op("pe", lambda e, pr=pr: e.transpose(ps_tr[:, pr * 128:(pr + 1) * 128], q_b[:, pr * 128:(pr + 1) * 128], ident_b[:]),
                 reads=[b_qb, bc], writes=[b_pstr], inc=False)
        for pr in range(2):
            P.op("pe", lambda e, pr=pr: e.transpose(ps_tr[:, 256 + pr * 128:256 + (pr + 1) * 128], k_b[:, pr * 128:(pr + 1) * 128], ident_b[:]),
                 reads=[b_kb, bc], writes=[b_pstr], inc=(pr == 1))
        for h in range(4):
            hp = slice((h % 2) * 64, (h % 2) * 64 + 64)
            if h % 2 == 0:
                P.op("dve", lambda e, qT_=qT_, h=h, hp=hp: e.tensor_copy(out=qT_[hp, h, :], in_=ps_tr[hp, (h // 2) * 128:(h // 2 + 1) * 128]), reads=[b_pstr], writes=[bqT_])
            else:
                P.op("act", lambda e, qT_=qT_, h=h, hp=hp: e.copy(out=qT_[hp, h, :], in_=ps_tr[hp, (h // 2) * 128:(h // 2 + 1) * 128]), reads=[b_pstr], writes=[bqT_])
        P.op("dve", lambda e: e.tensor_copy(out=kT[:, :, ns], in_=ps_tr[:, 256:512].rearrange("p (c t) -> p c t", c=2)), reads=[b_pstr], writes=[b_kT])
        P.op("dve", lambda e: e.tensor_scalar(out=ki2[:, 0, :], in0=at[:, 392:456], scalar1=st3[:, 2:3], scalar2=None, op0=ALU.mult), reads=[bat, b_st3], writes=[b_ki2])
        P.op("dve", lambda e: e.tensor_tensor(out=ki2[:, 0, :], in0=ki2[:, 0, :], in1=kig[:], op=ALU.mult), reads=[b_ki2, b_tab], writes=[b_ki2])
        rope_tm(P, "dve", ki2[:, 0:1, :], b_ki2, cosn, sinn, 1, rtmp, b_rtmp, b_tab)
        P.op("dve", lambda e: e.tensor_copy(out=ki2[:, 1, :], in_=ki2[:, 0, :]), reads=[b_ki2], writes=[b_ki2])
        rope_tm(P, "pool", qis[:], b_qis, cosn, sinn, 4, rtmp2, b_rtmp2, b_tab)
        P.op("dve", lambda e: e.tensor_scalar(out=wsc[:], in0=at[:, 456:460], scalar1=0.5 * 0.125, scalar2=None, op0=ALU.mult), reads=[bat], writes=[b_w4])
        P.op("dve", lambda e: e.tensor_scalar(out=lo4[:], in0=wsc[:], scalar1=0.0, scalar2=-BIG, op0=ALU.is_lt, op1=ALU.mult), reads=[b_w4], writes=[b_w4])
        P.op("dve", lambda e: e.tensor_scalar(out=hi4[:], in0=wsc[:], scalar1=0.0, scalar2=BIG, op0=ALU.is_ge, op1=ALU.mult), reads=[b_w4], writes=[b_w4])
        P.op("dve", lambda e: e.tensor_tensor(out=qis[:], in0=qis[:], in1=wsc[:, :].unsqueeze(2).to_broadcast([128, 4, 64]), op=ALU.mult),
             reads=[b_qis, b_w4], writes=[b_qis])
        qiT_, bqiT_ = qiT[n % 2], b_qiT[n % 2]
        for pr in range(2):
            P.op("pe", lambda e, pr=pr: e.transpose(ps_trf[:, pr * 128:(pr + 1) * 128], qis[:, 2 * pr:2 * pr + 2, :].rearrange("p h d -> p (h d)"), ident_f[:]),
                 reads=[b_qis, bc], writes=[b_pstrf], inc=False)
        P.op("pe", lambda e: e.transpose(ps_trf[:, 256:384], ki2[:].rearrange("p h d -> p (h d)"), ident_f[:]), reads=[b_ki2, bc], writes=[b_pstrf])
        P.op("act", lambda e, qiT_=qiT_: e.copy(out=qiT_[:], in_=ps_trf[:, 0:256].rearrange("p (c t) -> p c t", c=2)), reads=[b_pstrf], writes=[bqiT_])
        P.op("act", lambda e: e.copy(out=kiT[:, ns], in_=ps_trf[:, 256:384]), reads=[b_pstrf], writes=[b_kiT])
        nk = (n + 1) * 128
        if stage < 2:
            continue
        for g0 in range(0, nk, 512):
            wd = min(512, nk - g0)
            for h in range(4):
                psc, bpsc = ps_sc[sci % 2], b_pssc[sci % 2]
                cl, bcl = clp[sci % 2], b_clp[sci % 2]
                sci += 1
                hp = slice((h % 2) * 64, (h % 2) * 64 + 64)
                P.op("pe", lambda e, psc=psc, h=h, hp=hp, g0=g0, wd=wd, qiT_=qiT_: e.matmul(psc[:, 0:wd], lhsT=qiT_[hp, h // 2, :], rhs=kiT[hp, g0:g0 + wd],
                                                                                          start=True, stop=True),
                     reads=[bqiT_, b_kiT], writes=[bpsc])
                if h == 0:
                    P.op("dve", lambda e, psc=psc, h=h, g0=g0, wd=wd: e.tensor_scalar(out=score[:, g0:g0 + wd], in0=psc[:, 0:wd], scalar1=lo4[:, h:h + 1], scalar2=hi4[:, h:h + 1],
                                                                                     op0=ALU.max, op1=ALU.min), reads=[bpsc, b_w4], writes=[b_sc])
                else:
                    P.op("dve", lambda e, psc=psc, h=h, cl=cl, wd=wd: e.tensor_scalar(out=cl[:, 0:wd], in0=psc[:, 0:wd], scalar1=lo4[:, h:h + 1], scalar2=hi4[:, h:h + 1],
                                                                                     op0=ALU.max, op1=ALU.min), reads=[bpsc, b_w4], writes=[bcl])
                    P.op("pool", lambda e, cl=cl, g0=g0, wd=wd: e.tensor_tensor(out=score[:, g0:g0 + wd], in0=score[:, g0:g0 + wd], in1=cl[:, 0:wd], op=ALU.add),
                         reads=[bcl, b_sc], writes=[b_sc])
        P.op("dve", lambda e: e.tensor_tensor(out=score[:, ns], in0=score[:, ns], in1=CM[:], op=ALU.add), reads=[b_sc, b_tab], writes=[b_sc])
        if stage < 3:
            continue
        if n < 2:
            P.op("dve", lambda e: e.memset(lo[:], -1.0e29), writes=[b_bis])
        else:
            P.op("dve", lambda e: e.tensor_reduce(out=lo[:], in_=score[:, 0:n * 128], axis=AX.X, op=ALU.min), reads=[b_sc], writes=[b_bis])
            P.op("dve", lambda e: e.tensor_reduce(out=Wd[:], in_=score[:, 0:nk], axis=AX.X, op=ALU.max), reads=[b_sc], writes=[b_bis])
            P.op("dve", lambda e: e.tensor_scalar(out=Wd[:], in0=Wd[:], scalar1=lo[:, 0:1], scalar2=1.0e-6, op0=ALU.subtract, op1=ALU.add), reads=[b_bis], writes=[b_bis])
            P.op("dve", lambda e: e.tensor_scalar(out=Wk[:], in0=pw2[:], scalar1=Wd[:, 0:1], scalar2=None, op0=ALU.mult), reads=[b_bis, b_tab], writes=[b_bis])
            P.op("dve", lambda e: e.tensor_tensor(out=mid[:], in0=lo[:], in1=Wk[:, 0:1], op=ALU.add), reads=[b_bis], writes=[b_mid])
            for it in range(NBIS):
                P.op("dve", lambda e: e.tensor_scalar(out=junk[:, 0:nk], in0=score[:, 0:nk], scalar1=mid[:, 0:1], scalar2=0.0,
                                                      op0=ALU.is_ge, op1=ALU.add, accum_out=cnt[:, 0:1]), reads=[b_sc, b_mid], writes=[b_junk, b_cnt])
                P.op("dve", lambda e: e.tensor_scalar(out=dlt[:], in0=cnt[:], scalar1=255.5, scalar2=0.5, op0=ALU.is_ge, op1=ALU.subtract),
                     reads=[b_cnt], writes=[b_dlt])
                if it < NBIS - 1:
                    P.op("dve", lambda e, it=it: e.scalar_tensor_tensor(out=mid[:], in0=dlt[:], scalar=Wk[:, it:it + 1], in1=mid[:], op0=ALU.mult, op1=ALU.add),
                         reads=[b_dlt, b_bis, b_mid], writes=[b_mid])
                else:
                    P.op("dve", lambda e: e.tensor_scalar(out=dlt[:], in0=dlt[:], scalar1=0.5, scalar2=None, op0=ALU.subtract), reads=[b_dlt], writes=[b_dlt])
                    P.op("dve", lambda e, it=it: e.scalar_tensor_tensor(out=lo[:], in0=dlt[:], scalar=Wk[:, it:it + 1], in1=mid[:], op0=ALU.mult, op1=ALU.add),
                         reads=[b_dlt, b_bis, b_mid], writes=[b_bis])
        P.op("dve", lambda e: e.tensor_scalar(out=mask[:, 0:nk], in0=score[:, 0:nk], scalar1=lo[:, 0:1], scalar2=None, op0=ALU.is_ge), reads=[b_sc, b_bis], writes=[b_mask])
        if stage < 4:
            continue
        for kb in range(n + 1):
            ks = slice(kb * 128, (kb + 1) * 128)
            pS, bpS = ps_S[0], b_psS[0]
            p_, bp_ = pT[pti % 3], b_pT[pti % 3]
            pti += 1
            P.op("pe", lambda e, ks=ks: e.transpose(ps_mT[:, 256:384], mask[:, ks], ident_b[:]), reads=[b_mask, bc], writes=[b_pstr])
            if dbg.get('dsa_sub', 9) < 1:
                continue
            for h in dbg.get('heads', range(4)):
                hp = slice((h % 2) * 64, (h % 2) * 64 + 64)
                P.op("pe", lambda e, h=h, hp=hp, ks=ks, qT_=qT_: e.matmul(pS[:, h * 128:(h + 1) * 128], lhsT=kT[:, h // 2, ks], rhs=qT_[:, h, :], start=True, stop=True),
                     reads=[b_kT, bqT_], writes=[bpS], inc=(h == 3 or 'heads' in dbg))
            P.op("act", lambda e, p_=p_: e.activation(out=p_[:].rearrange("p h q -> p (h q)"), in_=pS[:, 0:512], func=AF.Exp, scale=0.125), reads=[bpS], writes=[bp_])
            if dbg.get('dsa_sub', 9) < 2:
                continue
            P.op("dve", lambda e, p_=p_: e.tensor_tensor(out=p_[:], in0=p_[:], in1=ps_mT[:, 256:384].unsqueeze(1).to_broadcast([128, 4, 128]), op=ALU.mult),
                 reads=[bp_, b_pstr], writes=[bp_])
            if dbg.get('dsa_sub', 9) < 3:
                continue
            for h in range(4):
                P.op("pe", lambda e, h=h, kb=kb, p_=p_, n=n: e.matmul(ps_o[:, h * 65:(h + 1) * 65], lhsT=p_[:, h, :], rhs=vaug[:, kb, h, :],
                                                                     start=(kb == 0 and h == 0), stop=(kb == n and h == 3), skip_group_check=True),
                     reads=[bp_, b_v], writes=[b_pso], inc=(h == 3))
        if dbg.get('dsa_sub', 9) < 4:
            continue
        po3 = ps_o[:, 0:260].rearrange("p (h d) -> p h d", h=4)
        P.op("dve", lambda e: e.reciprocal(out=rden[:].unsqueeze(2), in_=po3[:, :, 64:65]), reads=[b_pso], writes=[b_rden])
        P.op("dve", lambda e: e.tensor_tensor(out=yat[:].rearrange("p (h d) -> p h d", h=4), in0=po3[:, :, 0:64], in1=rden[:, :].unsqueeze(2).to_broadcast([128, 4, 64]), op=ALU.mult),
             reads=[b_pso, b_rden], writes=[b_yat])
        for pr in range(2):
            P.op("pe", lambda e, pr=pr: e.transpose(ps_tr[:, pr * 128:(pr + 1) * 128], yat[:, pr * 128:(pr + 1) * 128], ident_b[:]),
                 reads=[b_yat, bc], writes=[b_pstr], inc=(pr == 1))
        ya_, bya_ = yaT[n % 2], b_yaT[n % 2]
        P.op("act", lambda e, ya_=ya_: e.copy(out=ya_[:], in_=ps_tr[:, 0:256].rearrange("p (c t) -> p c t", c=2)), reads=[b_pstr], writes=[bya_])
        P.dma("sp", mixed_v[:, 4:6, ns], ya_[:], reads=[bya_])
    P.barrier()
    C.release(m)


SCRATCH = {
    "xT": ([8, 128, SEQ], F32),
    "z_tm": ([SEQ, 512], F32),
    "att_tm": ([SEQ, 460], F32),
    "xbcT": ([1024, SEQ], F32),
    "cqkvT": ([384, SEQ], BF16),
    "xlglT": ([512, SEQ], F32),
    "mixedT": ([1024, SEQ], BF16),
    "w_out_s": ([128, 8, D_MODEL], BF16),
}

PARAMS = {
    "ffn1_w13": [DEPTH, 128, 8, 2 * D_FF], "ffn1_w2": [DEPTH, 128, 22, D_MODEL], "norm_ffn1": [DEPTH, 128, 8],
    "ffn2_w13": [DEPTH, 128, 8, 2 * D_FF], "ffn2_w2": [DEPTH, 128, 22, D_MODEL], "norm_ffn2": [DEPTH, 128, 8],
    "norm_mix": [DEPTH, 128, 8], "w_in": [DEPTH, 128, 8, IN_COLS], "w_out": [DEPTH, 128, 8, D_MODEL],
    "ssd_conv_w": [DEPTH, 128, 8, 4], "ssd_conv_b": [DEPTH, 128, 8], "ssd_dt_bias": [DEPTH, 128, 8], "ssd_a_log": [DEPTH, 128, 8],
    "ssd_d": [DEPTH, 128, 512], "ssd_norm": [DEPTH, 128, 4],
    "cq_norm": [DEPTH, 128, 2], "ckv_norm": [DEPTH, 128, 1], "w_uq": [DEPTH, 128, 2, 256], "w_qidx": [DEPTH, 128, 2, 256], "w_ukv": [DEPTH, 128, 512],
    "q_norm": [DEPTH, 128, 64], "k_norm": [DEPTH, 128, 64], "kidx_norm": [DEPTH, 128, 64],
    "lru_conv_w": [DEPTH, 128, 2, 4], "lru_conv_b": [DEPTH, 128, 2], "lru_ba": [DEPTH, 128, 2], "lru_bi": [DEPTH, 128, 2], "lru_lambda": [DEPTH, 128, 2],
    "lru_wa": [DEPTH, 128, 2, 128], "lru_wi": [DEPTH, 128, 2, 128],
    "c_ident_f": [128, 128], "c_U": [128, 128], "c_SL": [128, 128], "c_cos": [128, 32, 8], "c_sin": [128, 32, 8], "c_cmask": [128, 128],
    "c_rdiv": [128, 3], "c_pw2": [128, NBIS],
}


def build_program(debug=None):
    debug = debug or {}
    nc = bass.Bass("TRN2", target_bir_lowering=False)
    P = Prog(nc)
    C = Ctx(nc)
    K = {'dbg': debug}
    x = nc.dram_tensor("x", [SEQ, D_MODEL], F32, kind="ExternalInput").ap()
    y = nc.dram_tensor("y", [SEQ, D_MODEL], F32, kind="ExternalOutput").ap()
    for name, shape in PARAMS.items():
        K[name] = nc.dram_tensor(name, list(shape), F32, kind="ExternalInput").ap()
    for name, (shape, dt) in SCRATCH.items():
        kind = "Internal"
        if name in debug.get("ext_in", ()):
            kind = "ExternalInput"
        elif name in debug.get("ext_out", ()):
            kind = "ExternalOutput"
        K[name] = nc.dram_tensor(name + "_scr", list(shape), dt, kind=kind).ap()

    K["b_const"] = bc = Buf("const")
    K["ones_bf"] = C.sb([128, 128], BF16, "ones")
    K["ones_f"] = C.sb([128, 128], F32, "onesf")
    K["ident_f"] = C.sb([128, 128], F32, "identf")
    K["ident_b"] = C.sb([128, 128], BF16, "identb")
    K["U_f"] = C.sb([128, 128], F32, "U")
    K["SL_f"] = C.sb([128, 128], F32, "SL")
    K["eps_sb"] = C.sb([128, 1], F32, "eps")
    P.op("dve", lambda e: e.memset(K["ones_bf"][:], 1.0), writes=[bc])
    P.op("dve", lambda e: e.memset(K["ones_f"][:], 1.0), writes=[bc])
    P.op("dve", lambda e: e.memset(K["eps_sb"][:], EPS), writes=[bc])
    P.dma("sp", K["ident_f"][:], K["c_ident_f"][:, :], writes=[bc])
    P.dma("sp", K["U_f"][:], K["c_U"][:, :], writes=[bc])
    P.dma("sp", K["SL_f"][:], K["c_SL"][:, :], writes=[bc])
    P.op("dve", lambda e: e.tensor_copy(out=K["ident_b"][:], in_=K["ident_f"][:]), reads=[bc], writes=[bc])
    P.barrier()

    only = debug.get("only")
    nl = debug.get("nl", DEPTH)
    for l in range(nl):
        last = (l == DEPTH - 1)
        if only in (None, "ffn1"):
            ffn_phase(P, C, K, l, 1, src_tm=(x if (l == 0 and "xT" not in debug.get("ext_in", ())) else None),
                      dst_tm=(y if only == "ffn1" else None))
        if only in (None, "inproj"):
            inproj_phase(P, C, K, l)
        if only in (None, "ssd"):
            ssd_phase(P, C, K, l)
        if only in (None, "lru"):
            lru_phase(P, C, K, l)
        if only in (None, "dsa"):
            dsa_phase(P, C, K, l)
        if only in (None, "ffn2"):
            ffn_phase(P, C, K, l, 2, dst_tm=(y if (last or only == "ffn2") else None), outproj=True)
    P.barrier()
    print("instructions", P.nins, "waits", P.nwait, "counts", P.cnt, flush=True)
    return nc


def host_layout(inputs):
    f = {}
    L = DEPTH
    f32 = np.float32

    def kchunk(w):
        Lk, Kd, N = w.shape
        return np.ascontiguousarray(w.reshape(Lk, Kd // 128, 128, N).transpose(0, 2, 1, 3))

    def pchunk(v):
        Lk, Cn = v.shape
        return np.ascontiguousarray(v.reshape(Lk, Cn // 128, 128).transpose(0, 2, 1))

    def bcast(v, n=128):
        return np.ascontiguousarray(np.broadcast_to(v[:, None, :], (v.shape[0], n, v.shape[1])))

    for which in (1, 2):
        f[f"ffn{which}_w13"] = kchunk(inputs[f"ffn{which}_w13"])
        f[f"ffn{which}_w2"] = kchunk(inputs[f"ffn{which}_w2"])
        f[f"norm_ffn{which}"] = pchunk(inputs[f"norm_ffn{which}"])
    f["norm_mix"] = pchunk(inputs["norm_mix"])
    f["w_in"] = kchunk(inputs["w_in"])
    f["w_out"] = kchunk(inputs["w_out"])
    cw = inputs["ssd_conv_w"]
    f["ssd_conv_w"] = np.ascontiguousarray(cw.reshape(L, 4, 8, 128).transpose(0, 3, 2, 1))
    f["ssd_conv_b"] = pchunk(inputs["ssd_conv_b"])
    f["ssd_dt_bias"] = bcast(inputs["ssd_dt_bias"])
    f["ssd_a_log"] = bcast(inputs["ssd_a_log"])
    f["ssd_d"] = bcast(np.repeat(inputs["ssd_d"], 64, axis=1))
    f["ssd_norm"] = pchunk(inputs["ssd_norm"])
    f["cq_norm"] = pchunk(inputs["cq_norm"])
    f["ckv_norm"] = pchunk(inputs["ckv_norm"])
    f["w_uq"] = kchunk(inputs["w_uq"])
    f["w_qidx"] = kchunk(inputs["w_qidx"])
    f["w_ukv"] = np.ascontiguousarray(inputs["w_ukv"])
    f["q_norm"] = bcast(inputs["q_norm"])
    f["k_norm"] = bcast(inputs["k_norm"])
    f["kidx_norm"] = bcast(inputs["kidx_norm"])
    lw = inputs["lru_conv_w"]
    f["lru_conv_w"] = np.ascontiguousarray(lw.reshape(L, 4, 2, 128).transpose(0, 3, 2, 1))
    f["lru_conv_b"] = pchunk(inputs["lru_conv_b"])
    f["lru_ba"] = pchunk(inputs["lru_ba"])
    f["lru_bi"] = pchunk(inputs["lru_bi"])
    f["lru_lambda"] = pchunk(inputs["lru_lambda"])
    for nm in ("lru_wa", "lru_wi"):
        w = inputs[nm]
        bd = np.zeros((L, 128, 2, 128), f32)
        for cc in range(2):
            for b in range(2):
                bd[:, b * 64:(b + 1) * 64, cc, b * 64:(b + 1) * 64] = w[:, 2 * cc + b]
        f[nm] = bd
    f["c_ident_f"] = np.eye(128, dtype=f32)
    kk = np.arange(128)
    f["c_U"] = (kk[:, None] <= kk[None, :]).astype(f32)
    f["c_SL"] = (kk[:, None] > kk[None, :]).astype(f32)
    inv = (np.float32(500000.0) ** (-(np.arange(8, dtype=f32) * np.float32(2.0) / np.float32(16)))).astype(f32)
    ang = (np.arange(SEQ, dtype=f32)[:, None] * inv[None, :]).astype(f32)
    f["c_cos"] = np.ascontiguousarray(np.cos(ang).astype(f32).reshape(32, 128, 8).transpose(1, 0, 2))
    f["c_sin"] = np.ascontiguousarray(np.sin(ang).astype(f32).reshape(32, 128, 8).transpose(1, 0, 2))
    f["c_cmask"] = np.where(kk[None, :] > kk[:, None], f32(-1.0e30), f32(0.0)).astype(f32)
    f["c_rdiv"] = np.ascontiguousarray(np.broadcast_to(np.array([1.0 / 256, 1.0 / 128, 1.0 / 64], f32)[None, :], (128, 3)))
    f["c_pw2"] = np.ascontiguousarray(np.broadcast_to((0.5 ** np.arange(1, NBIS + 1)).astype(f32)[None, :], (128, NBIS)))
    return f


def kernel(**inputs):
    inputs = {k: np.asarray(v) for k, v in inputs.items()}
    shared = host_layout(inputs)
    nc = build_program()
    x = np.ascontiguousarray(inputs["x"], dtype=np.float32)
    in_maps = []
    for b in range(NCORES):
        mm = dict(shared)
        mm["x"] = x[b]
        in_maps.append(mm)
    res = run_bass_kernel_spmd(nc, in_maps, core_ids=list(range(NCORES)))
    return np.stack([r["y"] for r in res.results], axis=0).astype(np.float32)
```
